# Optimizing a Trainium2 kernel written in Bass

```python
import math
import jax
import jax.numpy as jnp
from jax import lax
import numpy as np

D_MODEL = 1024
BATCH = 16
SEQ = 2048
DEPTH = 4

N_MIXERS = 2
N_ATTN_LAYERS = (DEPTH + N_MIXERS - 1) // N_MIXERS
N_SSM_LAYERS = DEPTH // N_MIXERS

HEAD_DIM = 64
N_Q_HEADS = D_MODEL // HEAD_DIM
N_KV_HEADS = 4
Q_PER_KV = N_Q_HEADS // N_KV_HEADS
WINDOW = 128
ATTN_BLOCK = WINDOW
Q_DIM = N_Q_HEADS * HEAD_DIM
KV_DIM = N_KV_HEADS * HEAD_DIM
QKV_DIM = Q_DIM + 2 * KV_DIM

SSM_GROUP = 16
N_SSM_GROUPS = D_MODEL // SSM_GROUP
SSM_STATE = 64
DT_MIN = 1e-3
DT_MAX = 1e-1

N_EXPERT_GROUPS = 4
EXPERTS_PER_GROUP = 8
N_EXPERTS = N_EXPERT_GROUPS * EXPERTS_PER_GROUP
TOP_K = 2
D_EXPERT = 256
MOE_BLOCK = 128

RMS_EPS = 1e-6
N_ADA = 6

kernel_name = 'hybrid_swa_sink_s5_hmoe'


def rms_norm(x, gain):
    xf = x.astype(jnp.float32)
    xf = xf * lax.rsqrt(jnp.mean(xf * xf, axis=-1, keepdims=True) + RMS_EPS)
    return (xf * gain.astype(jnp.float32)).astype(x.dtype)


def sliding_window_attention(h, w_qkv, q_gain, k_gain, sinks, w_o):
    B, S, D = h.shape
    nb = S // ATTN_BLOCK
    qkv = h @ w_qkv
    q = qkv[..., :Q_DIM].reshape(B, S, N_Q_HEADS, HEAD_DIM)
    k = qkv[..., Q_DIM:Q_DIM + KV_DIM].reshape(B, S, N_KV_HEADS, HEAD_DIM)
    v = qkv[..., Q_DIM + KV_DIM:].reshape(B, S, N_KV_HEADS, HEAD_DIM)
    q = rms_norm(q, q_gain) * (HEAD_DIM ** -0.5)
    k = rms_norm(k, k_gain)
    q = q.reshape(B, nb, ATTN_BLOCK, N_KV_HEADS, Q_PER_KV, HEAD_DIM)

    def with_prev(t):
        t = t.reshape(B, nb, ATTN_BLOCK, N_KV_HEADS, HEAD_DIM)
        prev = jnp.pad(t, ((0, 0), (1, 0), (0, 0), (0, 0), (0, 0)))[:, :-1]
        return jnp.concatenate([prev, t], axis=2)

    kb, vb = with_prev(k), with_prev(v)
    scores = jnp.einsum('bnqhgd,bnshd->bnhgqs', q, kb).astype(jnp.float32)
    qi = jnp.arange(ATTN_BLOCK)[:, None]
    sj = jnp.arange(2 * ATTN_BLOCK)[None, :]
    rel = ATTN_BLOCK + qi - sj
    band = (rel >= 0) & (rel < WINDOW)
    has_prev = (jnp.arange(nb) > 0)[:, None, None]
    valid = band[None] & (has_prev | (sj >= ATTN_BLOCK)[None])
    scores = jnp.where(valid[None, :, None, None], scores, -jnp.inf)
    sink = jnp.broadcast_to(
        sinks.astype(jnp.float32).reshape(1, 1, N_KV_HEADS, Q_PER_KV, 1, 1),
        scores.shape[:-1] + (1,))
    probs = jax.nn.softmax(jnp.concatenate([scores, sink], axis=-1), axis=-1)[..., :-1]
    out = jnp.einsum('bnhgqs,bnshd->bnqhgd', probs.astype(vb.dtype), vb)
    return out.reshape(B, S, Q_DIM) @ w_o


def s5_mixer(h, w_in, lam_re, lam_im, log_dt, b_re, b_im, c_re, c_im, d_skip, w_glu):
    B, S, D = h.shape
    f32 = jnp.float32
    u = (h @ w_in).astype(f32)
    ug = u.reshape(B, S, N_SSM_GROUPS, SSM_GROUP)
    dt = jnp.exp(log_dt.astype(f32))[:, None]
    lr, li = lam_re.astype(f32), lam_im.astype(f32)
    mag = jnp.exp(lr * dt)
    ab_re, ab_im = mag * jnp.cos(li * dt), mag * jnp.sin(li * dt)
    den = lr * lr + li * li
    n_re = ab_re - 1.0
    f_re = (n_re * lr + ab_im * li) / den
    f_im = (ab_im * lr - n_re * li) / den
    br, bi = b_re.astype(f32), b_im.astype(f32)
    bb_re = f_re[..., None] * br - f_im[..., None] * bi
    bb_im = f_re[..., None] * bi + f_im[..., None] * br
    bu_re = jnp.einsum('bsgc,gpc->sbgp', ug, bb_re)
    bu_im = jnp.einsum('bsgc,gpc->sbgp', ug, bb_im)
    a_re = jnp.broadcast_to(ab_re[None, None], (S, 1, N_SSM_GROUPS, SSM_STATE))
    a_im = jnp.broadcast_to(ab_im[None, None], (S, 1, N_SSM_GROUPS, SSM_STATE))

    def combine(left, right):
        al_re, al_im, xl_re, xl_im = left
        ar_re, ar_im, xr_re, xr_im = right
        return (ar_re * al_re - ar_im * al_im,
                ar_re * al_im + ar_im * al_re,
                ar_re * xl_re - ar_im * xl_im + xr_re,
                ar_re * xl_im + ar_im * xl_re + xr_im)

    _, _, xs_re, xs_im = lax.associative_scan(combine, (a_re, a_im, bu_re, bu_im), axis=0)
    y = (jnp.einsum('sbgp,gcp->bsgc', xs_re, c_re.astype(f32))
         - jnp.einsum('sbgp,gcp->bsgc', xs_im, c_im.astype(f32)))
    y = y.reshape(B, S, D) + d_skip.astype(f32) * u
    y = jax.nn.gelu(y).astype(h.dtype)
    z = y @ w_glu
    return z[..., :D] * jax.nn.sigmoid(z[..., D:])


def hierarchical_moe(h, w_group, b_group, w_expert, b_expert, w_gate, w_up, w_down):
    B, S, D = h.shape
    N = B * S
    xt = h.reshape(N, D)
    g_prob = jax.nn.softmax((xt @ w_group).astype(jnp.float32) + b_group.astype(jnp.float32), axis=-1)
    g_w, g_idx = lax.top_k(g_prob, 1)
    e_logits = ((xt @ w_expert).astype(jnp.float32) + b_expert.astype(jnp.float32)
                ).reshape(N, N_EXPERT_GROUPS, EXPERTS_PER_GROUP)
    e_sel = jnp.take_along_axis(e_logits, g_idx[:, :, None], axis=1)[:, 0]
    e_w, e_idx = lax.top_k(jax.nn.softmax(e_sel, axis=-1), TOP_K)
    gates = g_w * (e_w / jnp.sum(e_w, axis=-1, keepdims=True))
    experts = g_idx * EXPERTS_PER_GROUP + e_idx

    A = N * TOP_K
    flat_e = experts.reshape(A).astype(jnp.int32)
    flat_tok = jnp.repeat(jnp.arange(N, dtype=jnp.int32), TOP_K)
    flat_gate = gates.reshape(A)
    order = jnp.argsort(flat_e)
    s_e, s_tok, s_gate = flat_e[order], flat_tok[order], flat_gate[order]
    counts = jnp.zeros((N_EXPERTS,), jnp.int32).at[flat_e].add(1)
    starts = jnp.cumsum(counts) - counts
    padded = (counts + MOE_BLOCK - 1) // MOE_BLOCK * MOE_BLOCK
    pad_ends = jnp.cumsum(padded)
    pad_starts = pad_ends - padded
    dest = pad_starts[s_e] + jnp.arange(A, dtype=jnp.int32) - starts[s_e]
    n_blocks = -(-A // MOE_BLOCK) + N_EXPERTS
    P = n_blocks * MOE_BLOCK
    row_tok = jnp.zeros((P,), jnp.int32).at[dest].set(s_tok)
    row_gate = jnp.zeros((P,), jnp.float32).at[dest].set(s_gate)
    block_start = jnp.arange(n_blocks, dtype=jnp.int32) * MOE_BLOCK
    block_expert = jnp.minimum(jnp.searchsorted(pad_ends, block_start, side='right'),
                               N_EXPERTS - 1)
    xb = xt[row_tok].reshape(n_blocks, MOE_BLOCK, D)

    def expert_block(args):
        xblk, e = args
        hid = jax.nn.silu(xblk @ w_gate[e]) * (xblk @ w_up[e])
        return hid @ w_down[e]

    yb = lax.map(expert_block, (xb, block_expert))
    y_rows = yb.reshape(P, D) * row_gate[:, None].astype(yb.dtype)
    y = jax.ops.segment_sum(y_rows, row_tok, num_segments=N)
    return y.reshape(B, S, D)


def setup_inputs(seed: int = 0) -> dict:
    key = jax.random.key(seed)
    ks = iter(jax.random.split(key, 40))

    def nrm(shape, scale):
        return scale * jax.random.normal(next(ks), shape, jnp.float32)

    L, LA, LS = DEPTH, N_ATTN_LAYERS, N_SSM_LAYERS
    G, P, C = N_SSM_GROUPS, SSM_STATE, SSM_GROUP
    E, F, D = N_EXPERTS, D_EXPERT, D_MODEL
    lam_im = jnp.broadcast_to(math.pi * jnp.arange(P, dtype=jnp.float32), (LS, G, P))
    return {
        'x': nrm((BATCH, SEQ, D), 1.0),
        'c': nrm((BATCH, D), 1.0),
        'norm_mix': 1.0 + nrm((L, D), 0.02),
        'norm_ffn': 1.0 + nrm((L, D), 0.02),
        'w_ada': nrm((L, D, N_ADA * D), 0.5 * D ** -0.5),
        'b_ada': nrm((L, N_ADA * D), 0.02),
        'attn_w_qkv': nrm((LA, D, QKV_DIM), D ** -0.5),
        'attn_q_gain': 1.0 + nrm((LA, HEAD_DIM), 0.02),
        'attn_k_gain': 1.0 + nrm((LA, HEAD_DIM), 0.02),
        'attn_sinks': nrm((LA, N_Q_HEADS), 0.5),
        'attn_w_o': nrm((LA, Q_DIM, D), Q_DIM ** -0.5),
        'ssm_w_in': nrm((LS, D, D), D ** -0.5),
        'ssm_lam_re': -0.5 + nrm((LS, G, P), 0.01),
        'ssm_lam_im': lam_im,
        'ssm_log_dt': jax.random.uniform(next(ks), (LS, G), jnp.float32,
                                         math.log(DT_MIN), math.log(DT_MAX)),
        'ssm_b_re': nrm((LS, G, P, C), (2.0 * C) ** -0.5),
        'ssm_b_im': nrm((LS, G, P, C), (2.0 * C) ** -0.5),
        'ssm_c_re': nrm((LS, G, C, P), (2.0 * P) ** -0.5),
        'ssm_c_im': nrm((LS, G, C, P), (2.0 * P) ** -0.5),
        'ssm_d': nrm((LS, D), 1.0),
        'ssm_w_glu': nrm((LS, D, 2 * D), D ** -0.5),
        'moe_w_group': nrm((L, D, N_EXPERT_GROUPS), D ** -0.5),
        'moe_b_group': nrm((L, N_EXPERT_GROUPS), 0.01),
        'moe_w_expert': nrm((L, D, E), D ** -0.5),
        'moe_b_expert': nrm((L, E), 0.01),
        'moe_w_gate': nrm((L, E, D, F), D ** -0.5),
        'moe_w_up': nrm((L, E, D, F), D ** -0.5),
        'moe_w_down': nrm((L, E, F, D), F ** -0.5),
    }


def reference(x, c, norm_mix, norm_ffn, w_ada, b_ada,
              attn_w_qkv, attn_q_gain, attn_k_gain, attn_sinks, attn_w_o,
              ssm_w_in, ssm_lam_re, ssm_lam_im, ssm_log_dt, ssm_b_re, ssm_b_im,
              ssm_c_re, ssm_c_im, ssm_d, ssm_w_glu,
              moe_w_group, moe_b_group, moe_w_expert, moe_b_expert,
              moe_w_gate, moe_w_up, moe_w_down):
    c_act = jax.nn.silu(c)
    for layer in range(DEPTH):
        ada = (c_act @ w_ada[layer] + b_ada[layer])[:, None, :]
        sh1, sc1, g1, sh2, sc2, g2 = jnp.split(ada, N_ADA, axis=-1)
        h = rms_norm(x, norm_mix[layer]) * (1.0 + sc1) + sh1
        if layer % N_MIXERS == 0:
            i = layer // N_MIXERS
            mix = sliding_window_attention(h, attn_w_qkv[i], attn_q_gain[i], attn_k_gain[i],
                                           attn_sinks[i], attn_w_o[i])
        else:
            i = layer // N_MIXERS
            mix = s5_mixer(h, ssm_w_in[i], ssm_lam_re[i], ssm_lam_im[i], ssm_log_dt[i],
                           ssm_b_re[i], ssm_b_im[i], ssm_c_re[i], ssm_c_im[i],
                           ssm_d[i], ssm_w_glu[i])
        x = x + g1 * mix
        h = rms_norm(x, norm_ffn[layer]) * (1.0 + sc2) + sh2
        x = x + g2 * hierarchical_moe(h, moe_w_group[layer], moe_b_group[layer],
                                      moe_w_expert[layer], moe_b_expert[layer],
                                      moe_w_gate[layer], moe_w_up[layer], moe_w_down[layer])
    return x
```

```python
import numpy as np
from contextlib import ExitStack
import ml_dtypes
import concourse.bass as bass
import concourse.mybir as mybir
from concourse.bass_utils import run_bass_kernel_spmd

F32 = mybir.dt.float32
BF16 = mybir.dt.bfloat16
I32 = mybir.dt.int32
SPARSE = True
MOE_R0, MOE_R1 = 22, 21
AF = mybir.ActivationFunctionType
ALU = mybir.AluOpType
AX = mybir.AxisListType

D = 1024
T = 2048
L = 4
NE = 32
EPS = 1e-6
NCORES = 8
SEQ_PER_CORE = 2
NEG = -30000.0
NDS = 56


class Prog:
    def __init__(self, nc):
        self.nc = nc
        self.eng = {'pe': nc.tensor, 'act': nc.scalar, 'dve': nc.vector,
                    'pool': nc.gpsimd, 'sp': nc.sync}
        self.sems = {}
        for e in self.eng:
            self.sems['E' + e] = nc.alloc_semaphore(name='sem_' + e)
        self.ecnt = {e: 0 for e in self.eng}
        self.waited = {e: {} for e in self.eng}
        self.lw = {}
        self.rd = {}
        self.dval = [0] * NDS
        for i in range(NDS):
            self.sems['D%d' % i] = nc.alloc_semaphore(name='dsem%d' % i)
        self.dnext = {'sp': 0, 'pool': 0, 'act': 0}
        self.drange = {'sp': (0, NDS // 2), 'act': (0, NDS // 2), 'pool': (NDS // 2, NDS)}
        self.nwaits = 0

    def _deps(self, reads, writes):
        deps = {}

        def add(ev):
            if ev is None:
                return
            k, v = ev
            if deps.get(k, 0) < v:
                deps[k] = v
        for k in reads:
            add(self.lw.get(k))
        for k in writes:
            add(self.lw.get(k))
            for ev in self.rd.get(k, {}).items():
                add(ev)
        return deps

    def _wait(self, e, deps):
        w = self.waited[e]
        for k, v in deps.items():
            if w.get(k, 0) < v:
                self.eng[e].wait_ge(self.sems[k], v)
                w[k] = v
                self.nwaits += 1

    def _record(self, reads, writes, ev):
        k0, v0 = ev
        for k in writes:
            self.lw[k] = ev
            self.rd[k] = {}
        for k in reads:
            d = self.rd.setdefault(k, {})
            if d.get(k0, 0) < v0:
                d[k0] = v0

    def op(self, e, reads, writes, fn):
        deps = self._deps(reads, writes)
        if e == 'pe':
            deps.pop('Epe', None)
        self._wait(e, deps)
        inst = fn(self.eng[e])
        self.ecnt[e] += 1
        inst.then_inc(self.sems['E' + e], 1)
        self._record(reads, writes, ('E' + e, self.ecnt[e]))

    def dma(self, q, out, in_, reads, writes, **kw):
        deps = self._deps(reads, writes)
        lo, hi = self.drange[q]
        i = lo + self.dnext[q]
        self.dnext[q] = (self.dnext[q] + 1) % (hi - lo)
        if self.dval[i] > 0:
            k = 'D%d' % i
            if deps.get(k, 0) < self.dval[i]:
                deps[k] = self.dval[i]
        self._wait(q, deps)
        self.dval[i] += 16
        self.eng[q].dma_start(out=out, in_=in_, **kw).then_inc(self.sems['D%d' % i], 16)
        self._record(reads, writes, ('D%d' % i, self.dval[i]))

    def idma(self, q, fn, reads, writes):
        deps = self._deps(reads, writes)
        lo, hi = self.drange[q]
        i = lo + self.dnext[q]
        self.dnext[q] = (self.dnext[q] + 1) % (hi - lo)
        if self.dval[i] > 0:
            k = 'D%d' % i
            if deps.get(k, 0) < self.dval[i]:
                deps[k] = self.dval[i]
        self._wait(q, deps)
        self.dval[i] += 16
        fn(self.eng[q]).then_inc(self.sems['D%d' % i], 16)
        self._record(reads, writes, ('D%d' % i, self.dval[i]))

    def barrier(self):
        deps = {}
        for e in self.eng:
            if self.ecnt[e] > 0:
                deps['E' + e] = self.ecnt[e]
        for i in range(NDS):
            if self.dval[i] > 0:
                deps['D%d' % i] = self.dval[i]
        for e in self.eng:
            d = dict(deps)
            self._wait(e, d)
        self.lw = {}
        self.rd = {}


def mm(ps, lhsT, rhs, start, stop):
    return lambda pe: pe.matmul(ps, lhsT, rhs, start=start, stop=stop)


def build_program(n_layers=L, n_seq=SEQ_PER_CORE, dbg=None, layers=None):
    layers = list(range(n_layers)) if layers is None else layers
    n_layers = L
    nc = bass.Bass("TRN2", target_bir_lowering=False)
    P = Prog(nc)

    def din(name, shape, dt=F32):
        return nc.dram_tensor(name, list(shape), dt, kind="ExternalInput").ap()

    xT = din("xT", [SEQ_PER_CORE, D, T])
    cT = din("cT", [128, 8, 2])
    gmix = din("gmix", [128, L, 8])
    gffn = din("gffn", [128, L, 8])
    w_ada = din("w_ada", [L, 128, 8, 6 * D])
    b_ada = din("b_ada", [128, L, 48])
    wqkv = din("wqkv", [2, 128, 8, 1536])
    qgain = din("qgain", [128, 2])
    kgain = din("kgain", [128, 2])
    sinks = din("sinks", [2, 128, 2, 512])
    wo = din("wo", [2, 128, 8, D])
    maskb = din("maskb", [128, 2, 512], BF16)
    identb = din("identb", [128, 128], BF16)
    onesblk = din("onesblk", [128, 128], BF16)
    wr = din("wr", [L, 128, 8, 36])
    br = din("br", [1, L, 36])
    if not SPARSE:
        wgu = din("wgu", [L, NE, 128, 8, 512])
        wdn = din("wdn", [L, NE, 128, 2, D])
        sel = din("sel", [32, 32, 128])
    identf = din("identf", [128, 128])
    w_in = din("w_in", [2, 128, 8, D])
    w_glu = din("w_glu", [2, 128, 8, 2 * D])
    ssm_d = din("ssm_d", [128, 2, 8])
    lam_re = din("lam_re", [128, 2, 32])
    lam_im = din("lam_im", [128, 2, 32])
    log_dt = din("log_dt", [128, 2, 32])
    b_re = din("b_re", [128, 2, 32, 64])
    b_im = din("b_im", [128, 2, 32, 64])
    c_re = din("c_re", [128, 2, 32, 64])
    c_im = din("c_im", [128, 2, 32, 64])
    ident2 = din("ident2", [128, 64])
    wguA = din("wguA", [L * NE * 128, 2048])
    wguB = din("wguB", [L * NE * 128, 2048])
    wdn2 = din("wdn2", [L * NE * 128, 2048])
    trib = din("trib", [128, 128], BF16)
    toki = din("toki", [128, 16], I32)
    rinit = din("rinit", [128, 512], I32)
    b128 = din("b128", [128, 64])
    pidx = din("pidx", [128, 1])
    HD = nc.dram_tensor("HD", [2176, 1024], BF16, kind="Internal").ap()
    YD = nc.dram_tensor("YD", [8192, 1024], F32, kind="Internal").ap()
    ROWT = nc.dram_tensor("ROWT", [8192, 8], I32, kind="Internal").ap()
    yT = nc.dram_tensor("yT", [SEQ_PER_CORE, D, T], F32, kind="ExternalOutput").ap()
    ssm_scr = nc.dram_tensor("ssm_scr", [2, 8, 128, 8704], BF16, kind="Internal").ap()

    _uid = [0]
    _regs = {}

    def sb(name, shape, dt):
        _uid[0] += 1
        return nc.sbuf_tensor("%s_%d" % (name, _uid[0]), shape, dt)
    ps_ = nc.psum_tensor

    with ExitStack() as es:
        AKS = es.enter_context(sb("AKS", [128, 2, 3, 8, 32], F32))
        ADA = es.enter_context(sb("ADA", [128, L, 48, 2], F32))
        SCL = es.enter_context(sb("SCL", [128, L, 2, 2, 8], F32))
        ONESB = es.enter_context(sb("ONESB", [128, 128], BF16))
        ONEBLK = es.enter_context(sb("ONEBLK", [128, 128], BF16))
        IDB = es.enter_context(sb("IDB", [128, 128], BF16))
        TRIB = es.enter_context(sb("TRIB", [128, 128], BF16))
        IDF = es.enter_context(sb("IDF", [128, 128], F32))
        ONEF = es.enter_context(sb("ONEF", [1, 128], F32))
        GMIX = es.enter_context(sb("GMIX", [128, L, 8], F32))
        GFFN = es.enter_context(sb("GFFN", [128, L, 8], F32))
        BR = es.enter_context(sb("BR", [1, L, 36], F32))
        PS0 = es.enter_context(ps_("PS0", [128, 512], F32))
        PS1 = es.enter_context(ps_("PS1", [128, 512], F32))
        PS2 = es.enter_context(ps_("PS2", [128, 512], F32))
        PS3 = es.enter_context(ps_("PS3", [128, 512], F32))
        PS4 = es.enter_context(ps_("PS4", [128, 512], F32))
        PS5 = es.enter_context(ps_("PS5", [128, 512], F32))
        PS6 = es.enter_context(ps_("PS6", [128, 512], F32))
        PS7 = es.enter_context(ps_("PS7", [128, 512], F32))
        PS = [PS0, PS1, PS2, PS3, PS4, PS5, PS6, PS7]

        P.op('dve', [], ['ONESB'], lambda e: e.memset(ONESB[:], 1.0))
        P.op('dve', [], ['ONEF'], lambda e: e.memset(ONEF[:], 1.0))
        P.dma('sp', ONEBLK[:], onesblk[:, :], [], ['ONEBLK'])
        P.dma('sp', IDB[:], identb[:, :], [], ['IDB'])
        P.dma('sp', TRIB[:], trib[:, :], [], ['TRIB'])
        P.dma('sp', IDF[:], identf[:, :], [], ['IDF'])
        P.dma('sp', GMIX[:], gmix[:, :, :], [], ['GMIX'])
        P.dma('sp', GFFN[:], gffn[:, :, :], [], ['GFFN'])
        P.dma('sp', BR[:], br[:, :, :], [], ['BR'])

        def ada_phase(prep_gens):
            with ExitStack() as es:
                CT = es.enter_context(sb("CT", [128, 8, 2], F32))
                CA = es.enter_context(sb("CA", [128, 8, 2], F32))
                BADA = es.enter_context(sb("BADA", [128, L, 48], F32))
                WA0 = es.enter_context(sb("WA0", [128, 8, 768], F32))
                WA1 = es.enter_context(sb("WA1", [128, 8, 768], F32))
                WA = [WA0, WA1]
                P.dma('sp', CT[:], cT[:, :, :], [], ['CT'])
                P.dma('sp', BADA[:], b_ada[:, :, :], [], ['BADA'])
                P.op('act', ['CT'], ['CA'], lambda e: e.activation(out=CA[:], in_=CT[:], func=AF.Silu))

                def ada_block(it):
                    l, cb = it // 8, it % 8
                    W = WA[it % 2]
                    wk = 'WA%d' % (it % 2)
                    pk = 'PS%d' % (it % 2)
                    pst = PS[it % 2]
                    P.dma('sp', W[:], w_ada[l, :, :, cb * 768:(cb + 1) * 768], [], [wk])
                    for j in range(6):
                        for kc in range(8):
                            P.op('pe', [wk, 'CA'], [pk],
                                 mm(pst[:, 2 * j:2 * j + 2], W[:, kc, j * 128:(j + 1) * 128],
                                    CA[:, kc, :], kc == 0, kc == 7))
                    pv = pst[:, 0:12].rearrange("p (j s) -> p j s", s=2)
                    bb = BADA[:, l, cb * 6:(cb + 1) * 6].unsqueeze(2).to_broadcast([128, 6, 2])
                    P.op('dve', [pk, 'BADA'], ['ADA%d_%d' % (l, cb)],
                         lambda e: e.tensor_tensor(out=ADA[:, l, cb * 6:(cb + 1) * 6, :], in0=pv, in1=bb, op=ALU.add))
                nblk = n_layers * 8
                pos = 0
                for g in prep_gens:
                    for _ in g:
                        for _k in range(2):
                            if pos < nblk:
                                ada_block(pos)
                                pos += 1
                while pos < nblk:
                    ada_block(pos)
                    pos += 1
                allk = ['ADA%d_%d' % (l, cb) for l in range(n_layers) for cb in range(8)]
                for l in range(n_layers):
                    for w in range(2):
                        G = GMIX if w == 0 else GFFN
                        off = 8 if w == 0 else 32
                        for s in range(2):
                            P.op('dve', allk + ['GMIX', 'GFFN'], ['SCL'],
                                 lambda e, l=l, w=w, s=s, G=G, off=off: e.scalar_tensor_tensor(
                                     out=SCL[:, l, w, s, :], in0=ADA[:, l, off:off + 8, s], scalar=1.0,
                                     in1=G[:, l, :], op0=ALU.add, op1=ALU.mult))
                P.barrier()

        def emit_ssm_prep(i):
            TWO_PI = 6.283185307179586
            with ExitStack() as es2:
                def t32(nm):
                    return es2.enter_context(sb(nm, [128, 32], F32))
                LR, LI, LDT, DTt, LRD, LID, MAG = [t32(n) for n in ("LR", "LI", "LDT", "DTt", "LRD", "LID", "MAG")]
                U1, KF, FR, NEGm, SINV, COSV = [t32(n) for n in ("U1", "KF", "FR", "NEGm", "SINV", "COSV")]
                ABR, ABI, DEN, RDN, NRE, A1, A2, FRE, FIM = [t32(n) for n in ("ABR", "ABI", "DEN", "RDN", "NRE", "A1", "A2", "FRE", "FIM")]
                KI = es2.enter_context(sb("KI", [128, 32], mybir.dt.int32))
                NPI = es2.enter_context(sb("NPI", [128, 1], F32))
                ID2 = es2.enter_context(sb("ID2", [128, 64], F32))
                DV = es2.enter_context(sb("DV", [128, 2, 8], F32))
                PWR = es2.enter_context(sb("PWR", [128, 9, 32], F32))
                PWI = es2.enter_context(sb("PWI", [128, 9, 32], F32))
                CRt = es2.enter_context(sb("CRt", [128, 32, 64], F32))
                CIt = es2.enter_context(sb("CIt", [128, 32, 64], F32))
                BBR = es2.enter_context(sb("BBR", [128, 32, 64], F32))
                BBI = es2.enter_context(sb("BBI", [128, 32, 64], F32))
                esh = ExitStack()
                BRt = esh.enter_context(sb("BRt", [128, 32, 64], F32))
                BIt = esh.enter_context(sb("BIt", [128, 32, 64], F32))
                T1 = esh.enter_context(sb("T1", [128, 32, 64], F32))
                T2 = esh.enter_context(sb("T2", [128, 32, 64], F32))
                P.dma('sp', LR[:], lam_re[:, i, :], [], ['LR'])
                P.dma('sp', LI[:], lam_im[:, i, :], [], ['LI'])
                P.dma('sp', LDT[:], log_dt[:, i, :], [], ['LDT'])
                P.dma('sp', BRt[:], b_re[:, i, :, :], [], ['BRt'])
                P.dma('sp', BIt[:], b_im[:, i, :, :], [], ['BIt'])
                P.dma('sp', CRt[:], c_re[:, i, :, :], [], ['CRt'])
                P.dma('sp', CIt[:], c_im[:, i, :, :], [], ['CIt'])
                P.dma('sp', ID2[:], ident2[:, :], [], ['ID2'])
                P.dma('sp', DV[:], ssm_d[:, :, :], [], ['DV'])
                P.op('dve', [], ['NPI'], lambda e: e.memset(NPI[:], -3.141592653589793))

                def tt_(out, a_, b_, op, rk, wk, eng='dve'):
                    P.op(eng, rk, wk, lambda e: e.tensor_tensor(out=out, in0=a_, in1=b_, op=op))

                P.op('act', ['LDT'], ['DTt'], lambda e: e.activation(out=DTt[:], in_=LDT[:], func=AF.Exp))
                tt_(LRD[:], LR[:], DTt[:], ALU.mult, ['LR', 'DTt'], ['LRD'])
                tt_(LID[:], LI[:], DTt[:], ALU.mult, ['LI', 'DTt'], ['LID'])
                P.op('act', ['LRD'], ['MAG'], lambda e: e.activation(out=MAG[:], in_=LRD[:], func=AF.Exp))

                def sincos(dst, dk, phase):
                    P.op('dve', ['LID'], ['U1'], lambda e: e.tensor_scalar(
                        out=U1[:], in0=LID[:], scalar1=1.0 / TWO_PI, scalar2=phase, op0=ALU.mult, op1=ALU.add))
                    P.op('dve', ['U1'], ['KI'], lambda e: e.tensor_copy(out=KI[:], in_=U1[:]))
                    P.op('dve', ['KI'], ['KF'], lambda e: e.tensor_copy(out=KF[:], in_=KI[:]))
                    tt_(FR[:], U1[:], KF[:], ALU.subtract, ['U1', 'KF'], ['FR'])
                    P.op('dve', ['FR'], ['NEGm'], lambda e: e.tensor_scalar(
                        out=NEGm[:], in0=FR[:], scalar1=0.0, scalar2=None, op0=ALU.is_lt))
                    tt_(FR[:], FR[:], NEGm[:], ALU.add, ['FR', 'NEGm'], ['FR'])
                    P.op('act', ['FR', 'NPI'], [dk], lambda e: e.activation(
                        out=dst[:], in_=FR[:], func=AF.Sin, scale=TWO_PI, bias=NPI[:, 0:1]))
                sincos(SINV, 'SINV', 0.5)
                sincos(COSV, 'COSV', 0.75)
                tt_(ABR[:], MAG[:], COSV[:], ALU.mult, ['MAG', 'COSV'], ['ABR'])
                tt_(ABI[:], MAG[:], SINV[:], ALU.mult, ['MAG', 'SINV'], ['ABI'])
                tt_(A1[:], LR[:], LR[:], ALU.mult, ['LR'], ['A1'])
                tt_(A2[:], LI[:], LI[:], ALU.mult, ['LI'], ['A2'])
                tt_(DEN[:], A1[:], A2[:], ALU.add, ['A1', 'A2'], ['DEN'])
                P.op('dve', ['DEN'], ['RDN'], lambda e: e.reciprocal(out=RDN[:], in_=DEN[:]))
                P.op('dve', ['ABR'], ['NRE'], lambda e: e.tensor_scalar(
                    out=NRE[:], in0=ABR[:], scalar1=-1.0, scalar2=None, op0=ALU.add))
                tt_(A1[:], NRE[:], LR[:], ALU.mult, ['NRE', 'LR'], ['A1'])
                tt_(A2[:], ABI[:], LI[:], ALU.mult, ['ABI', 'LI'], ['A2'])
                tt_(A1[:], A1[:], A2[:], ALU.add, ['A1', 'A2'], ['A1'])
                tt_(FRE[:], A1[:], RDN[:], ALU.mult, ['A1', 'RDN'], ['FRE'])
                tt_(A1[:], ABI[:], LR[:], ALU.mult, ['ABI', 'LR'], ['A1'])
                tt_(A2[:], NRE[:], LI[:], ALU.mult, ['NRE', 'LI'], ['A2'])
                tt_(A1[:], A1[:], A2[:], ALU.subtract, ['A1', 'A2'], ['A1'])
                tt_(FIM[:], A1[:], RDN[:], ALU.mult, ['A1', 'RDN'], ['FIM'])
                frb = FRE[:].unsqueeze(2).to_broadcast([128, 32, 64])
                fib = FIM[:].unsqueeze(2).to_broadcast([128, 32, 64])
                tt_(T1[:], BRt[:], frb, ALU.mult, ['BRt', 'FRE'], ['T1'])
                tt_(T2[:], BIt[:], fib, ALU.mult, ['BIt', 'FIM'], ['T2'], eng='pool')
                tt_(BBR[:], T1[:], T2[:], ALU.subtract, ['T1', 'T2'], ['BBR'])
                tt_(T1[:], BIt[:], frb, ALU.mult, ['BIt', 'FRE'], ['T1'])
                tt_(T2[:], BRt[:], fib, ALU.mult, ['BRt', 'FIM'], ['T2'], eng='pool')
                tt_(BBI[:], T1[:], T2[:], ALU.add, ['T1', 'T2'], ['BBI'])
                P.op('dve', [], ['PWR'], lambda e: e.memset(PWR[:, 0, :], 1.0))
                P.op('dve', [], ['PWI'], lambda e: e.memset(PWI[:, 0, :], 0.0))
                for k in range(8):
                    tt_(A1[:], PWR[:, k, :], ABR[:], ALU.mult, ['PWR', 'ABR'], ['A1'])
                    tt_(A2[:], PWI[:, k, :], ABI[:], ALU.mult, ['PWI', 'ABI'], ['A2'])
                    tt_(PWR[:, k + 1, :], A1[:], A2[:], ALU.subtract, ['A1', 'A2'], ['PWR'])
                    tt_(A1[:], PWR[:, k, :], ABI[:], ALU.mult, ['PWR', 'ABI'], ['A1'])
                    tt_(A2[:], PWI[:, k, :], ABR[:], ALU.mult, ['PWI', 'ABR'], ['A2'])
                    tt_(PWI[:, k + 1, :], A1[:], A2[:], ALU.add, ['A1', 'A2'], ['PWI'])
                P.op('dve', ['PWR'], ['AKS'], lambda e: e.tensor_copy(out=AKS[:, i, 0, 0, :], in_=PWR[:, 8, :]))
                P.op('dve', ['PWI'], ['AKS'], lambda e: e.tensor_copy(out=AKS[:, i, 1, 0, :], in_=PWI[:, 8, :]))
                P.op('dve', ['PWI'], ['AKS'], lambda e: e.tensor_scalar(
                    out=AKS[:, i, 2, 0, :], in0=PWI[:, 8, :], scalar1=-1.0, scalar2=None, op0=ALU.mult))
                for k in range(7):
                    tt_(A1[:], AKS[:, i, 0, k, :], AKS[:, i, 0, k, :], ALU.mult, ['AKS'], ['A1'])
                    tt_(A2[:], AKS[:, i, 1, k, :], AKS[:, i, 1, k, :], ALU.mult, ['AKS'], ['A2'])
                    tt_(U1[:], AKS[:, i, 0, k, :], AKS[:, i, 1, k, :], ALU.mult, ['AKS'], ['U1'])
                    tt_(AKS[:, i, 0, k + 1, :], A1[:], A2[:], ALU.subtract, ['A1', 'A2'], ['AKS'])
                    P.op('dve', ['U1'], ['AKS'], lambda e, k=k: e.tensor_scalar(
                        out=AKS[:, i, 1, k + 1, :], in0=U1[:], scalar1=2.0, scalar2=None, op0=ALU.mult))
                    P.op('dve', ['U1'], ['AKS'], lambda e, k=k: e.tensor_scalar(
                        out=AKS[:, i, 2, k + 1, :], in0=U1[:], scalar1=-2.0, scalar2=None, op0=ALU.mult))
                P.barrier()
                esh.close()
                TA2 = [es2.enter_context(sb("TA%d" % b_, [128, 9, 4, 64], F32)) for b_ in range(2)]
                TB2 = [es2.enter_context(sb("TB%d" % b_, [128, 9, 4, 64], F32)) for b_ in range(2)]
                SBb2 = [es2.enter_context(sb("SBb%d" % b_, [128, 2, 8, 4, 64], BF16)) for b_ in range(2)]
                CAb2 = [es2.enter_context(sb("CAb%d" % b_, [128, 2, 9, 4, 64], BF16)) for b_ in range(2)]
                OUTB = [es2.enter_context(sb("OUTB%d" % b, [128, 8704], BF16)) for b in range(2)]
                yield
                def stage_a(cc):
                    j0 = cc * 4
                    TA, TB, SBb, CAb = TA2[cc % 2], TB2[cc % 2], SBb2[cc % 2], CAb2[cc % 2]
                    sfx = '_%d' % (cc % 2)
                    ob = OUTB[cc % 2]
                    ok = 'OUTB%d' % (cc % 2)
                    pwr8 = PWR[:, 0:8, j0:j0 + 4].unsqueeze(3).to_broadcast([128, 8, 4, 64])
                    pwi8 = PWI[:, 0:8, j0:j0 + 4].unsqueeze(3).to_broadcast([128, 8, 4, 64])
                    pwr9 = PWR[:, 0:9, j0:j0 + 4].unsqueeze(3).to_broadcast([128, 9, 4, 64])
                    pwi9 = PWI[:, 0:9, j0:j0 + 4].unsqueeze(3).to_broadcast([128, 9, 4, 64])
                    bbr = BBR[:, j0:j0 + 4, :].unsqueeze(1).to_broadcast([128, 8, 4, 64])
                    bbi = BBI[:, j0:j0 + 4, :].unsqueeze(1).to_broadcast([128, 8, 4, 64])
                    cr9 = CRt[:, j0:j0 + 4, :].unsqueeze(1).to_broadcast([128, 9, 4, 64])
                    ci9 = CIt[:, j0:j0 + 4, :].unsqueeze(1).to_broadcast([128, 9, 4, 64])
                    tt_(TA[:, 0:8], bbr, pwr8, ALU.mult, ['BBR', 'PWR'], ['TA' + sfx])
                    tt_(TB[:, 0:8], bbi, pwi8, ALU.mult, ['BBI', 'PWI'], ['TB' + sfx], eng='pool')
                    tt_(SBb[:, 0], TA[:, 0:8], TB[:, 0:8], ALU.subtract, ['TA' + sfx, 'TB' + sfx], ['SBb0' + sfx])
                    tt_(TA[:, 0:8], bbi, pwr8, ALU.mult, ['BBI', 'PWR'], ['TA' + sfx])
                    tt_(TB[:, 0:8], bbr, pwi8, ALU.mult, ['BBR', 'PWI'], ['TB' + sfx])
                    tt_(SBb[:, 1], TA[:, 0:8], TB[:, 0:8], ALU.add, ['TA' + sfx, 'TB' + sfx], ['SBb1' + sfx])
                    tt_(TA[:], cr9, pwr9, ALU.mult, ['CRt', 'PWR'], ['TA' + sfx])
                    tt_(TB[:], ci9, pwi9, ALU.mult, ['CIt', 'PWI'], ['TB' + sfx])
                    tt_(CAb[:, 0], TA[:], TB[:], ALU.subtract, ['TA' + sfx, 'TB' + sfx], ['CAb0' + sfx])
                    tt_(TA[:], cr9, pwi9, ALU.mult, ['CRt', 'PWI'], ['TA' + sfx])
                    tt_(TB[:], ci9, pwr9, ALU.mult, ['CIt', 'PWR'], ['TB' + sfx], eng='pool')
                    P.op('dve', ['TA' + sfx, 'TB' + sfx], ['CAb1' + sfx], lambda e: e.scalar_tensor_tensor(
                        out=CAb[:, 1], in0=TA[:], scalar=-1.0, in1=TB[:], op0=ALU.mult, op1=ALU.subtract))
                def stage_b(cc):
                    j0 = cc * 4
                    TA, TB, SBb, CAb = TA2[cc % 2], TB2[cc % 2], SBb2[cc % 2], CAb2[cc % 2]
                    sfx = '_%d' % (cc % 2)
                    ob = OUTB[cc % 2]
                    ok = 'OUTB%d' % (cc % 2)
                    for tau in range(8):
                        pi = 2 + (tau % 2)
                        for q in range(4):
                            h2, w = q // 2, q % 2
                            hs = slice(64 * h2, 64 * h2 + 64)
                            P.op('pe', ['SBb0' + sfx, 'IDB'], ['PS%d_%d' % (pi, h2)],
                                 mm(PS[pi][hs, w * 256:w * 256 + 128], SBb[:, 0, tau, q, :], IDB[:], True, True))
                            P.op('pe', ['SBb1' + sfx, 'IDB'], ['PS%d_%d' % (pi, h2)],
                                 mm(PS[pi][hs, w * 256 + 128:w * 256 + 256], SBb[:, 1, tau, q, :], IDB[:], True, True))
                        P.op('act', ['PS%d_0' % pi, 'PS%d_1' % pi], [ok],
                             lambda e, ob=ob, tau=tau, pi=pi: e.copy(out=ob[:, tau * 512:(tau + 1) * 512], in_=PS[pi][:, 0:512]))
                    for tau in range(8):
                        for h2 in range(2):
                            hs = slice(64 * h2, 64 * h2 + 64)
                            for w in range(2):
                                q = 2 * h2 + w
                                P.op('pe', ['SBb0' + sfx, 'CAb0' + sfx], ['PS4_%d' % h2],
                                     mm(PS4[hs, tau * 64:(tau + 1) * 64], SBb[:, 0, 0, q, :], CAb[:, 0, tau, q, :], w == 0, False))
                                P.op('pe', ['SBb1' + sfx, 'CAb1' + sfx], ['PS4_%d' % h2],
                                     mm(PS4[hs, tau * 64:(tau + 1) * 64], SBb[:, 1, 0, q, :], CAb[:, 1, tau, q, :], False, w == 1))
                    P.op('dve', ['PS4_0', 'PS4_1', 'ID2', 'DV'], [ok], lambda e, ob=ob, cc=cc: e.scalar_tensor_tensor(
                        out=ob[:, 4096:4160], in0=ID2[:], scalar=DV[:, i, cc:cc + 1], in1=PS4[:, 0:64],
                        op0=ALU.mult, op1=ALU.add))
                    P.op('act', ['PS4_0', 'PS4_1'], [ok], lambda e, ob=ob: e.copy(out=ob[:, 4160:4608], in_=PS4[:, 64:512]))
                    for r in range(2):
                        P.op('act', ['CAb%d' % r + sfx], [ok], lambda e, ob=ob, r=r: e.copy(
                            out=ob[:, 4608 + r * 2048:4608 + (r + 1) * 2048].rearrange("p (t q c) -> p t q c", t=8, q=4),
                            in_=CAb[:, r, 1:9]))
                    P.dma('act', ssm_scr[i, cc, :, :], ob[:], [ok], ['SCR%d_%d' % (i, cc)])
                stage_a(0)
                for cc in range(8):
                    if cc + 1 < 8:
                        stage_a(cc + 1)
                    stage_b(cc)
                    yield
                P.barrier()

        ada_phase([emit_ssm_prep(i_) for i_ in sorted(set(l_ // 2 for l_ in layers if l_ % 2 == 1))])

        def ada_vec(l, idx, s, kc):
            return ADA[:, l, idx * 8 + kc, s:s + 1]

        def emit_dispatch(l, es, GTS, OHS, OHF):
            def t(nm, w, dt=F32):
                return es.enter_context(sb(nm, [128, w], dt))
            CNT, X1, U2, KF2, FR2, NG2, FL, PC, PEND, PST, ONE32 = [t(n, 32) for n in
                ("CNT", "X1", "U2", "KF2", "FR2", "NG2", "FL", "PC", "PEND", "PST", "ONE32")]
            KI2 = t("KI2", 32, I32)
            VAL = t("VAL", 32)
            VAL1 = t("VAL1", 32)
            JNK = t("JNK", 32)
            M8d = t("M8d", 8)
            DP1 = t("DP1", 32)
            GSEL = t("GSEL", 32)
            DST, DQ, DU, DKF, DFR, DNG, D2 = [t(n, 32) for n in ("DST", "DQ", "DU", "DKF", "DFR", "DNG", "D2")]
            DKI = t("DKI", 32, I32)
            D2I = t("D2I", 32, I32)
            PAYI = es.enter_context(sb("PAYI", [128, 16, 2, 8], I32))
            TOKI = t("TOKI", 16, I32)
            RIN = t("RIN", 512, I32)
            B128 = t("B128", 64)
            PIDX = t("PIDX", 1)
            CMP = es.enter_context(sb("CMP", [128, 64, 32], F32))
            BE, SK, EM, IW = [t(n, 64) for n in ("BE", "SK", "EM", "IW")]
            P.dma('sp', TOKI[:], toki[:, :], [], ['TOKI'])
            P.dma('sp', RIN[:], rinit[:, :], [], ['RIN'])
            P.dma('sp', B128[:], b128[:, :], [], ['B128'])
            P.dma('sp', PIDX[:], pidx[:, :], [], ['PIDX'])
            P.op('dve', [], ['ONE32'], lambda e: e.memset(ONE32[:], 1.0))
            ohk = ['OHS%d' % n for n in range(16)]
            for n in range(16):
                P.op('pe', ['ONESB', 'OHS%d' % n], ['PS3'], mm(PS3[:, 0:32], ONESB[:], OHS[:, n, :], n == 0, n == 15))
            P.op('dve', ['PS3'], ['CNT'], lambda e: e.tensor_copy(out=CNT[:], in_=PS3[:, 0:32]))

            def floor128(dst, dk, srcap, sk, U, KI, KF, FRc, NG, pre):
                P.op('dve', [sk], [pre + 'U'], lambda e: e.tensor_scalar(out=U[:], in0=srcap, scalar1=1.0 / 128, scalar2=None, op0=ALU.mult))
                P.op('dve', [pre + 'U'], [pre + 'KI'], lambda e: e.tensor_copy(out=KI[:], in_=U[:]))
                P.op('dve', [pre + 'KI'], [pre + 'KF'], lambda e: e.tensor_copy(out=KF[:], in_=KI[:]))
                P.op('dve', [pre + 'U', pre + 'KF'], [pre + 'FR'], lambda e: e.tensor_tensor(out=FRc[:], in0=U[:], in1=KF[:], op=ALU.subtract))
                P.op('dve', [pre + 'FR'], [pre + 'NG'], lambda e: e.tensor_scalar(out=NG[:], in0=FRc[:], scalar1=0.0, scalar2=None, op0=ALU.is_lt))
                P.op('dve', [pre + 'KF', pre + 'NG'], [dk], lambda e: e.tensor_tensor(out=dst[:], in0=KF[:], in1=NG[:], op=ALU.subtract))
            P.op('dve', ['CNT'], ['X1'], lambda e: e.tensor_scalar(out=X1[:], in0=CNT[:], scalar1=127.0, scalar2=None, op0=ALU.add))
            floor128(FL, 'FL', X1[:], 'X1', U2, KI2, KF2, FR2, NG2, 'a')
            P.op('dve', ['FL'], ['PC'], lambda e: e.tensor_scalar(out=PC[:], in0=FL[:], scalar1=128.0, scalar2=None, op0=ALU.mult))
            P.op('dve', ['PC', 'ONE32'], ['PEND'], lambda e: e.tensor_tensor_scan(
                out=PEND[:], data0=ONE32[:], data1=PC[:], initial=0.0, op0=ALU.mult, op1=ALU.add))
            P.op('dve', ['PC', 'PEND'], ['PST'], lambda e: e.scalar_tensor_tensor(
                out=PST[:], in0=PC[:], scalar=-1.0, in1=PEND[:], op0=ALU.mult, op1=ALU.add))
            VA = es.enter_context(sb("VA", [128, 16, 32], F32))
            VB = es.enter_context(sb("VB", [128, 16, 32], F32))
            MH = es.enter_context(sb("MH", [128, 16, 32], F32))
            ML = es.enter_context(sb("ML", [128, 16, 32], F32))
            TQ = es.enter_context(sb("TQ", [128, 16, 32], F32))
            for n in range(16):
                first = True
                for n2 in range(n):
                    P.op('pe', ['ONESB', 'OHS%d' % n2], ['PS4'], mm(PS4[:, n * 32:(n + 1) * 32], ONESB[:], OHS[:, n2, :], first, False))
                    first = False
                P.op('pe', ['TRIB', 'OHS%d' % n], ['PS4'], mm(PS4[:, n * 32:(n + 1) * 32], TRIB[:], OHS[:, n, :], first, True))
            ofk = ['OHF%d' % n for n in range(16)]
            gtk = ['GTS%d' % n for n in range(16)]

            def b16(ap2):
                return ap2.unsqueeze(2).to_broadcast([128, 16, 32])
            DP3 = DP1[:].rearrange("p (n k) -> p n k", k=2)
            GS3 = GSEL[:].rearrange("p (n k) -> p n k", k=2)
            DHI, DLO, GHI, GLO = [t(nm, 16) for nm in ("DHI", "DLO", "GHI", "GLO")]
            P.op('dve', ['PS4', 'PST'], ['VA'], lambda e: e.tensor_tensor(
                out=VA[:], in0=PS4[:].rearrange("p (n c) -> p n c", n=16), in1=PST[:].unsqueeze(1).to_broadcast([128, 16, 32]), op=ALU.add))
            P.op('dve', ['VA'] + ofk, ['VA'], lambda e: e.scalar_tensor_tensor(
                out=VA[:], in0=VA[:], scalar=1.0, in1=OHF[:], op0=ALU.add, op1=ALU.mult))
            P.op('dve', ['VA'], ['DHI'], lambda e: e.tensor_reduce(out=DHI[:], in_=VA[:], axis=AX.X, op=ALU.max))
            P.op('dve', ['VA', 'DHI'], ['MH'], lambda e: e.tensor_tensor(out=MH[:], in0=VA[:], in1=b16(DHI[:]), op=ALU.is_equal))
            P.op('dve', ['MH', 'VA'], ['TQ'], lambda e: e.tensor_tensor(out=TQ[:], in0=MH[:], in1=VA[:], op=ALU.mult))
            P.op('dve', ['VA', 'TQ'], ['VB'], lambda e: e.tensor_tensor(out=VB[:], in0=VA[:], in1=TQ[:], op=ALU.subtract))
            P.op('dve', ['VB'], ['DLO'], lambda e: e.tensor_reduce(out=DLO[:], in_=VB[:], axis=AX.X, op=ALU.max))
            P.op('dve', ['VB', 'DLO'], ['ML'], lambda e: e.tensor_tensor(out=ML[:], in0=VB[:], in1=b16(DLO[:]), op=ALU.is_equal))
            P.op('dve', ['MH'] + gtk, ['TQ'], lambda e: e.tensor_tensor(out=TQ[:], in0=MH[:], in1=GTS[:], op=ALU.mult))
            P.op('dve', ['TQ'], ['GHI'], lambda e: e.tensor_reduce(out=GHI[:], in_=TQ[:], axis=AX.X, op=ALU.add))
            P.op('dve', ['ML', 'TQ'] + gtk, ['TQ'], lambda e: e.tensor_tensor(out=TQ[:], in0=ML[:], in1=GTS[:], op=ALU.mult))
            P.op('dve', ['TQ'], ['GLO'], lambda e: e.tensor_reduce(out=GLO[:], in_=TQ[:], axis=AX.X, op=ALU.add))
            P.op('dve', ['DHI'], ['DP1'], lambda e: e.tensor_copy(out=DP3[:, :, 0], in_=DHI[:]))
            P.op('dve', ['DLO', 'DP1'], ['DP1'], lambda e: e.tensor_copy(out=DP3[:, :, 1], in_=DLO[:]))
            P.op('dve', ['GHI'], ['GSEL'], lambda e: e.tensor_copy(out=GS3[:, :, 0], in_=GHI[:]))
            P.op('dve', ['GLO', 'GSEL'], ['GSEL'], lambda e: e.tensor_copy(out=GS3[:, :, 1], in_=GLO[:]))
            dpk = ['DP1']
            gsk = ['GSEL']
            P.op('dve', dpk, ['DST'], lambda e: e.tensor_scalar(out=DST[:], in0=DP1[:], scalar1=-1.0, scalar2=None, op0=ALU.add))
            P.op('dve', ['DST'], ['DSTI'], lambda e: e.tensor_copy(out=DSTI[:], in_=DST[:]))
            floor128(DQ, 'DQ', DST[:], 'DST', DU, DKI, DKF, DFR, DNG, 'b')
            P.op('dve', ['DQ', 'DST'], ['D2'], lambda e: e.scalar_tensor_tensor(
                out=D2[:], in0=DQ[:], scalar=-128.0, in1=DST[:], op0=ALU.mult, op1=ALU.add))
            P.op('dve', ['D2', 'DQ'], ['D2'], lambda e: e.scalar_tensor_tensor(
                out=D2[:], in0=D2[:], scalar=64.0, in1=DQ[:], op0=ALU.mult, op1=ALU.add))
            P.op('dve', ['D2'], ['D2I'], lambda e: e.tensor_copy(out=D2I[:], in_=D2[:]))
            P.op('dve', [], ['PAY0', 'PAY1'], lambda e: e.memset(PAYI[:], 0))
            P.op('dve', ['TOKI', 'PAY0'], ['PAY0'], lambda e: e.tensor_copy(
                out=PAYI[:, :, :, 0], in_=TOKI[:].unsqueeze(2).to_broadcast([128, 16, 2])))
            P.op('dve', gsk + ['PAY1'], ['PAY1'], lambda e: e.tensor_copy(
                out=PAYI[:].bitcast(F32)[:, :, :, 1], in_=GSEL[:].rearrange("p (n k) -> p n k", k=2)))
            P.dma('sp', ROWT.rearrange("(p x) c -> p (x c)", p=128), RIN[:], ['RIN'], ['ROWT'])
            if 'rowt' not in _regs:
                _regs['rowt'] = nc.gpsimd.alloc_register(name="rowtmax_reg")
                nc.gpsimd.reg_mov(_regs['rowt'], 8191)
            for n in range(16):
                for k in range(2):
                    P.idma('pool', lambda g, n=n, k=k: g.indirect_dma_start(
                        out=ROWT[:, :], out_offset=bass.IndirectOffsetOnAxis(ap=D2I[:, 2 * n + k:2 * n + k + 1], axis=0),
                        in_=PAYI[:, n, k, :], in_offset=None, bounds_check=_regs['rowt'], oob_is_err=False),
                        ['D2I', 'PAY0', 'PAY1', 'ROWT'], ['ROWTs%d_%d' % (n, k)])
            P.dma('sp', IDXG[:].rearrange("p b c -> p (b c)"), ROWT.rearrange("(p x) c -> p (x c)", p=128), ['ROWT'] + ['ROWTs%d_%d' % (n, k) for n in range(16) for k in range(2)], ['IDXG'])
            P.op('dve', ['PEND', 'B128'], ['CMP'], lambda e: e.tensor_tensor(
                out=CMP[:], in0=PEND[:].unsqueeze(1).to_broadcast([128, 64, 32]),
                in1=B128[:].unsqueeze(2).to_broadcast([128, 64, 32]), op=ALU.is_le))
            P.op('dve', ['CMP'], ['BE'], lambda e: e.tensor_reduce(out=BE[:], in_=CMP[:], axis=AX.X, op=ALU.add))
            P.op('dve', ['BE'], ['BE'], lambda e: e.tensor_scalar(out=BE[:], in0=BE[:], scalar1=31.0, scalar2=None, op0=ALU.min))
            P.op('dve', [], ['SK'], lambda e: e.memset(SK[:], 0.0))
            P.op('dve', ['BE', 'SK'], ['SK'], lambda e: e.tensor_tensor(out=SK[:, 1:64], in0=BE[:, 1:64], in1=BE[:, 0:63], op=ALU.is_equal))
            for rb in (MOE_R0, MOE_R0 + MOE_R1):
                P.op('dve', ['SK'], ['SK'], lambda e, rb=rb: e.memset(SK[:, rb:rb + 1], 0.0))
            P.op('dve', ['B128', 'PEND'], ['EM'], lambda e: e.tensor_scalar(out=EM[:], in0=B128[:], scalar1=PEND[:, 31:32], scalar2=None, op0=ALU.is_ge))
            P.op('dve', ['SK', 'EM'], ['SK'], lambda e: e.tensor_tensor(out=SK[:], in0=SK[:], in1=EM[:], op=ALU.max))
            P.op('dve', ['BE', 'PIDX'], ['IW'], lambda e: e.tensor_scalar(
                out=IW[:], in0=BE[:], scalar1=128.0, scalar2=PIDX[:, 0:1], op0=ALU.mult, op1=ALU.add))
            P.op('dve', ['IW', 'SK'], ['IW'], lambda e: e.scalar_tensor_tensor(
                out=IW[:], in0=SK[:], scalar=1.0e6, in1=IW[:], op0=ALU.mult, op1=ALU.add))
            P.op('dve', ['IW'], ['IDXW'], lambda e: e.tensor_scalar(
                out=IDXW[:], in0=IW[:], scalar1=float(4096 * l), scalar2=None, op0=ALU.add))

        def emit_norm(l, s, which, router=None):
            shift_idx = 0 if which == 0 else 3
            with ExitStack() as es:
                SQ2b = [es.enter_context(sb("SQ%d" % b_, [128, 8, 512], BF16)) for b_ in range(2)]
                SD2b = [es.enter_context(sb("SD%d" % b_, [128, 512], F32)) for b_ in range(2)]
                RS2b = [es.enter_context(sb("RS%d" % b_, [128, 512], F32)) for b_ in range(2)]
                TMP0 = es.enter_context(sb("TMP0", [128, 512], F32))
                TMP1 = es.enter_context(sb("TMP1", [128, 512], F32))
                TMP = [TMP0, TMP1]
                cnt = 0
                if which == 1:
                    HL2 = [es.enter_context(sb("HL%d" % b_, [128, 8, 512], BF16)) for b_ in range(2)]
                    WRF = es.enter_context(sb("WRF", [128, 8, 36], F32))
                    WRH = es.enter_context(sb("WRH", [128, 8, 36], BF16))
                    WRL = es.enter_context(sb("WRL", [128, 8, 36], BF16))
                    L36 = es.enter_context(sb("L36", [128, 36], F32))
                    RT = es.enter_context(sb("RT", [128, 16], F32))
                    OG = es.enter_context(sb("OG", [128, 4], F32))
                    PEN = es.enter_context(sb("PEN", [128, 4], F32))
                    GE = es.enter_context(sb("GE", [128, 4], F32))
                    LM = es.enter_context(sb("LM", [128, 32], F32))
                    M8 = es.enter_context(sb("M8", [128, 8], F32))
                    SELM = es.enter_context(sb("SELM", [128, 32], F32))
                    EX = es.enter_context(sb("EX", [128, 32], F32))
                    EXS = es.enter_context(sb("EXS", [128, 32], F32))
                    GATES = es.enter_context(sb("GATES", [128, 32], F32))
                    if SPARSE:
                        GTS = es.enter_context(sb("GTS", [128, 16, 32], F32))
                        OHS = es.enter_context(sb("OHS", [128, 16, 32], BF16))
                        OHF = es.enter_context(sb("OHF", [128, 16, 32], F32))
                        HROW = [es.enter_context(sb("HROW%d" % b, [128, 1024], BF16)) for b in range(2)]
                    P.dma('sp', WRF[:], wr[l, :, :, :], [], ['WRF'])
                    P.op('dve', ['WRF'], ['WRH'], lambda e: e.tensor_copy(out=WRH[:], in_=WRF[:]))
                    P.op('dve', ['WRF', 'WRH'], ['WRL'],
                         lambda e: e.tensor_tensor(out=WRL[:], in0=WRF[:], in1=WRH[:], op=ALU.subtract))

                    L4 = es.enter_context(sb("L4", [128, 4, 36], F32))
                    R4 = es.enter_context(sb("R4", [128, 8, 4], F32))
                    LGS = es.enter_context(sb("LGS", [128, 4, 4], F32))
                    GE4 = es.enter_context(sb("GE4", [128, 4, 4], F32))
                    OG4 = es.enter_context(sb("OG4", [128, 4, 4], F32))
                    PEN4 = es.enter_context(sb("PEN4", [128, 4, 4], F32))
                    LM4 = es.enter_context(sb("LM4", [128, 4, 32], F32))
                    MK1 = es.enter_context(sb("MK1", [128, 4, 32], F32))
                    LM2 = es.enter_context(sb("LM2", [128, 4, 32], F32))
                    LMS = es.enter_context(sb("LMS", [128, 4, 32], F32))
                    EX4 = es.enter_context(sb("EX4", [128, 4, 32], F32))
                    EXS4 = es.enter_context(sb("EXS4", [128, 4, 32], F32))

                    def emit_router(l, blk):
                        n4 = slice(blk * 4, blk * 4 + 4)
                        for tt in range(4):
                            n = blk * 4 + tt
                            ts_ = slice(n * 128, (n + 1) * 128)
                            tl = slice(tt * 128, (tt + 1) * 128)
                            pc = slice(tt * 64, tt * 64 + 36)
                            first = True
                            for kc in range(8):
                                for (A, ak, asl, Wt, wk) in ((HT, 'HT%d_%d' % (kc, blk), ts_, WRH, 'WRH'),
                                                             (HL2[blk % 2], 'HL%d_%d' % (blk % 2, kc), tl, WRH, 'WRH'),
                                                             (HT, 'HT%d_%d' % (kc, blk), ts_, WRL, 'WRL')):
                                    P.op('pe', [ak, wk], ['PS3'],
                                         mm(PS3[:, pc], A[:, kc, asl], Wt[:, kc, :], first, False))
                                    first = False
                            P.op('pe', ['ONEF', 'BR'], ['PS3'],
                                 mm(PS3[:, pc], ONEF[0:1, :], BR[0:1, l, :], False, True))
                            PSB = PS7[:].bitcast(BF16)
                            for kc in range(8):
                                P.op('pe', ['HT%d_%d' % (kc, blk), 'IDB'], ['PS7'],
                                     lambda pe, kc=kc, ts_=ts_: pe.transpose(PSB[:, kc * 128:(kc + 1) * 128], HT[:, kc, ts_], IDB[:]))
                            hb = n % 2
                            P.op('act', ['PS7'], ['HROW%d' % hb], lambda e, hb=hb: e.copy(out=HROW[hb][:], in_=PSB))
                            P.dma('sp', HD[n * 128:(n + 1) * 128, :], HROW[hb][:], ['HROW%d' % hb], ['HD'])

                        def bc(ap2, w):
                            return ap2.unsqueeze(2).to_broadcast([128, 4, w])
                        GM, GS, M1, M2, SS, DEN, RDEN = [R4[:, j, :] for j in range(7)]
                        okk = ['OHF%d' % n for n in range(blk * 4, blk * 4 + 4)]
                        gkk = ['GTS%d' % n for n in range(blk * 4, blk * 4 + 4)]
                        ohk = ['OHS%d' % n for n in range(blk * 4, blk * 4 + 4)]
                        P.op('dve', ['PS3'], ['L4'], lambda e: e.tensor_copy(
                            out=L4[:], in_=PS3[:, 0:256].rearrange("p (t c) -> p t c", t=4)[:, :, 0:36]))
                        P.op('dve', ['L4'], ['GM'], lambda e: e.tensor_reduce(out=GM, in_=L4[:, :, 0:4], axis=AX.X, op=ALU.max))
                        P.op('dve', ['L4', 'GM'], ['LGS'], lambda e: e.tensor_tensor(out=LGS[:], in0=L4[:, :, 0:4], in1=bc(GM, 4), op=ALU.subtract))
                        P.op('act', ['LGS'], ['GE4'], lambda e: e.activation(out=GE4[:], in_=LGS[:], func=AF.Exp))
                        P.op('dve', ['GE4'], ['GS'], lambda e: e.tensor_reduce(out=GS, in_=GE4[:], axis=AX.X, op=ALU.add))
                        P.op('dve', ['L4', 'GM'], ['OG4'], lambda e: e.tensor_tensor(out=OG4[:], in0=L4[:, :, 0:4], in1=bc(GM, 4), op=ALU.is_equal))
                        P.op('dve', ['OG4'], ['PEN4'], lambda e: e.tensor_scalar(
                            out=PEN4[:], in0=OG4[:], scalar1=-1.0, scalar2=1e30, op0=ALU.add, op1=ALU.mult))
                        P.op('dve', ['L4', 'PEN4'], ['LM4'], lambda e: e.tensor_tensor(
                            out=LM4[:].rearrange("p t (g e) -> p t g e", g=4),
                            in0=L4[:, :, 4:36].rearrange("p t (g e) -> p t g e", g=4),
                            in1=PEN4[:].unsqueeze(3).to_broadcast([128, 4, 4, 8]), op=ALU.add))
                        P.op('dve', ['LM4'], ['M1'], lambda e: e.tensor_reduce(out=M1, in_=LM4[:], axis=AX.X, op=ALU.max))
                        P.op('dve', ['LM4', 'M1'], ['MK1'], lambda e: e.tensor_tensor(out=MK1[:], in0=LM4[:], in1=bc(M1, 32), op=ALU.is_equal))
                        P.op('dve', ['MK1', 'LM4'], ['LM2'], lambda e: e.scalar_tensor_tensor(
                            out=LM2[:], in0=MK1[:], scalar=-1e30, in1=LM4[:], op0=ALU.mult, op1=ALU.add))
                        P.op('dve', ['LM2'], ['M2'], lambda e: e.tensor_reduce(out=M2, in_=LM2[:], axis=AX.X, op=ALU.max))
                        P.op('dve', ['LM4', 'M2'], okk, lambda e: e.tensor_tensor(out=OHF[:, n4, :], in0=LM4[:], in1=bc(M2, 32), op=ALU.is_ge))
                        P.op('act', okk, ohk, lambda e: e.copy(out=OHS[:, n4, :], in_=OHF[:, n4, :]))
                        P.op('dve', ['LM4', 'M1'], ['LMS'], lambda e: e.tensor_tensor(out=LMS[:], in0=LM4[:], in1=bc(M1, 32), op=ALU.subtract))
                        P.op('act', ['LMS'], ['EX4'], lambda e: e.activation(out=EX4[:], in_=LMS[:], func=AF.Exp))
                        P.op('dve', ['EX4'] + okk, ['EXS4'], lambda e: e.tensor_tensor(out=EXS4[:], in0=EX4[:], in1=OHF[:, n4, :], op=ALU.mult))
                        P.op('dve', ['EXS4'], ['SS'], lambda e: e.tensor_reduce(out=SS, in_=EXS4[:], axis=AX.X, op=ALU.add))
                        P.op('dve', ['SS', 'GS'], ['DEN'], lambda e: e.tensor_tensor(out=DEN, in0=SS, in1=GS, op=ALU.mult))
                        P.op('dve', ['DEN'], ['RDEN'], lambda e: e.reciprocal(out=RDEN, in_=DEN))
                        P.op('dve', ['EXS4', 'RDEN'], gkk, lambda e: e.tensor_tensor(out=GTS[:, n4, :], in0=EXS4[:], in1=bc(RDEN, 32), op=ALU.mult))
                for blk in range(4):
                    cs = slice(blk * 512, (blk + 1) * 512)
                    SQ, SD, RS = SQ2b[blk % 2], SD2b[blk % 2], RS2b[blk % 2]
                    sqk, sdk, rsk = 'SQ%d_' % (blk % 2), 'SD%d' % (blk % 2), 'RS%d' % (blk % 2)
                    psn = PS[1 + (blk % 2)]
                    psk = 'PS%d' % (1 + (blk % 2))
                    for kc in range(8):
                        P.op('act', ['XT', 'XTL%d' % kc], [sqk + str(kc)],
                             lambda e, kc=kc, cs=cs, SQ=SQ: e.activation(out=SQ[:, kc, :], in_=XT[:, kc, cs],
                                                                         func=AF.Square))
                    for kc in range(8):
                        P.op('pe', [sqk + str(kc), 'ONESB'], [psk],
                             mm(psn[:], ONESB[:], SQ[:, kc, :], kc == 0, kc == 7))
                    P.op('act', [psk], [sdk],
                         lambda e, SD=SD, psn=psn: e.activation(out=SD[:], in_=psn[:], func=AF.Sqrt,
                                                                scale=1.0 / D, bias=EPSB[:, 0:1]))
                    P.op('dve', [sdk], [rsk], lambda e, SD=SD, RS=RS: e.reciprocal(out=RS[:], in_=SD[:]))
                    for kc in range(8):
                        tk = 'TMP%d' % (cnt % 2)
                        TT = TMP[cnt % 2]
                        cnt += 1
                        P.op('dve', ['XT', 'XTL%d' % kc, rsk, 'SCL'], [tk],
                             lambda e, kc=kc, cs=cs, TT=TT, RS=RS: e.scalar_tensor_tensor(
                                 out=TT[:], in0=XT[:, kc, cs], scalar=SCL[:, l, which, s, kc:kc + 1],
                                 in1=RS[:], op0=ALU.mult, op1=ALU.mult))
                        P.op('act', [tk, 'ADA'], ['HT%d_%d' % (kc, blk)],
                             lambda e, kc=kc, cs=cs, TT=TT: e.activation(
                                 out=HT[:, kc, cs], in_=TT[:], func=AF.Identity,
                                 bias=ada_vec(l, shift_idx, s, kc), scale=1.0))
                        if which == 1:
                            P.op('dve', [tk, 'ADA', 'HT%d_%d' % (kc, blk)], ['HL%d_%d' % (blk % 2, kc)],
                                 lambda e, kc=kc, cs=cs, TT=TT: e.scalar_tensor_tensor(
                                     out=HL2[blk % 2][:, kc, :], in0=TT[:], scalar=ada_vec(l, shift_idx, s, kc),
                                     in1=HT[:, kc, cs], op0=ALU.add, op1=ALU.subtract))
                    if which == 1 and blk >= 1:
                        emit_router(l, blk - 1)
                if which == 1:
                    emit_router(l, 3)
                if which == 1 and SPARSE:
                    emit_dispatch(l, es, GTS, OHS, OHF)
                P.barrier()

        with ExitStack() as es:
            EPSB = es.enter_context(sb("EPSB", [128, 4], F32))
            XT = es.enter_context(sb("XT", [128, 8, T], F32))
            HT = es.enter_context(sb("HT", [128, 8, T], BF16))
            P.op('dve', [], ['EPSB'], lambda e: e.memset(EPSB[:, 0:1], EPS))
            P.op('dve', [], ['EPSB'], lambda e: e.memset(EPSB[:, 1:2], 64.0 * EPS))
            P.op('dve', [], ['EPSB'], lambda e: e.memset(EPSB[:, 2:3], 0.0))
            P.barrier()

            def emit_attn(i, l, s):
                with ExitStack() as es:
                    WQ = es.enter_context(sb("WQ", [128, 8, 1536], BF16))
                    WO = es.enter_context(sb("WO", [128, 8, D], BF16))
                    QT = es.enter_context(sb("QT", [128, 8, 512], BF16))
                    KT = es.enter_context(sb("KT", [128, 2, T], BF16))
                    V = es.enter_context(sb("V", [128, 16, 256], BF16))
                    OT = es.enter_context(sb("OT", [128, 2, 4, 512], BF16))
                    E0 = es.enter_context(sb("E0", [128, 512], BF16))
                    E1 = es.enter_context(sb("E1", [128, 512], BF16))
                    E2 = es.enter_context(sb("E2", [128, 512], BF16))
                    E3 = es.enter_context(sb("E3", [128, 512], BF16))
                    QG = es.enter_context(sb("QG", [128, 2], F32))
                    KG = es.enter_context(sb("KG", [128, 2], F32))
                    XS = es.enter_context(sb("XS", [128, 2, 512], F32))
                    MB = es.enter_context(sb("MB", [128, 2, 512], BF16))
                    SQ2 = es.enter_context(sb("SQ2", [128, 512], BF16))
                    SD2 = es.enter_context(sb("SD2", [128, 512], F32))
                    RS2 = es.enter_context(sb("RS2", [128, 512], F32))
                    DS = es.enter_context(sb("DS", [128, 512], F32))
                    RD = es.enter_context(sb("RD", [128, 512], F32))
                    EB = [E0, E1, E2, E3]
                    for kc in range(8):
                        P.dma('pool', WQ[:, kc, :], wqkv[i, :, kc, :], [], ['WQ%d' % kc])
                    for kc in range(8):
                        P.dma('pool', WO[:, kc, :], wo[i, :, kc, :], [], ['WO%d' % kc])
                    P.dma('sp', QG[:], qgain[:, :], [], ['QG'])
                    P.dma('sp', KG[:], kgain[:, :], [], ['KG'])
                    P.dma('sp', XS[:], sinks[i, :, :, :], [], ['XSraw'])
                    P.dma('sp', MB[:], maskb[:, :, :], [], ['MB'])
                    P.op('act', ['XSraw'], ['XS'], lambda e: e.activation(out=XS[:], in_=XS[:], func=AF.Exp))
                    pq = 0
                    ecnt = 0
                    for blk in range(4):
                        cs = slice(blk * 512, (blk + 1) * 512)
                        hkeys = ['HT%d_%d' % (kc, blk) for kc in range(8)]
                        for c in range(10):
                            pk = 'PS%d' % (pq % 2)
                            pst = PS[pq % 2]
                            pq += 1
                            for kc in range(8):
                                P.op('pe', ['WQ%d' % kc] + hkeys, [pk],
                                     mm(pst[:], WQ[:, kc, c * 128:(c + 1) * 128], HT[:, kc, cs], kc == 0, kc == 7))
                            P.op('act', [pk], ['SQ2'],
                                 lambda e, pst=pst: e.activation(out=SQ2[:], in_=pst[:], func=AF.Square))
                            P.op('pe', ['SQ2', 'ONEBLK'], ['PS2'], mm(PS2[:], ONEBLK[:], SQ2[:], True, True))
                            if c < 8:
                                P.op('act', ['PS2'], ['SD2'],
                                     lambda e: e.activation(out=SD2[:], in_=PS2[:], func=AF.Sqrt,
                                                            scale=1.0, bias=EPSB[:, 1:2]))
                            else:
                                P.op('act', ['PS2'], ['SD2'],
                                     lambda e: e.activation(out=SD2[:], in_=PS2[:], func=AF.Sqrt,
                                                            scale=1.0 / 64, bias=EPSB[:, 0:1]))
                            P.op('dve', ['SD2'], ['RS2'], lambda e: e.reciprocal(out=RS2[:], in_=SD2[:]))
                            if c < 8:
                                P.op('dve', [pk, 'RS2', 'QG'], ['QT%d' % c],
                                     lambda e, pst=pst, c=c: e.scalar_tensor_tensor(
                                         out=QT[:, c, :], in0=pst[:], scalar=QG[:, i:i + 1], in1=RS2[:],
                                         op0=ALU.mult, op1=ALU.mult))
                            else:
                                P.op('dve', [pk, 'RS2', 'KG'], ['KT%d_%d' % (c - 8, blk)],
                                     lambda e, pst=pst, c=c, cs=cs: e.scalar_tensor_tensor(
                                         out=KT[:, c - 8, cs], in0=pst[:], scalar=KG[:, i:i + 1], in1=RS2[:],
                                         op0=ALU.mult, op1=ALU.mult))
                        for tt in range(4):
                            n = blk * 4 + tt
                            pk = 'PS%d' % (pq % 2)
                            pst = PS[pq % 2]
                            pq += 1
                            for kc in range(8):
                                P.op('pe', ['WQ%d' % kc] + hkeys, [pk],
                                     mm(pst[:, 0:256], HT[:, kc, n * 128:(n + 1) * 128], WQ[:, kc, 1280:1536],
                                        kc == 0, kc == 7))
                            P.op('act', [pk], ['V%d' % n],
                                 lambda e, pst=pst, n=n: e.copy(out=V[:, n, :], in_=pst[:, 0:256]))
                        items = []
                        for tt in range(4):
                            n = blk * 4 + tt
                            for i2 in range(2):
                                for hh in range(2):
                                    cks = [1] if n == 0 else [0, 1]
                                    for ci, ck in enumerate(cks):
                                        items.append((tt, n, i2, hh, ck, ci == 0, ci == len(cks) - 1))

                        def emit_scores(item, ebuf, psb):
                            tt, n, i2, hh, ck, first, last = item
                            nk = n - 1 + ck
                            hs = slice(64 * hh, 64 * hh + 64)
                            pst = PS[3 + psb]
                            pk = 'PS%d' % (3 + psb)
                            rhs = QT[hs, 4 * i2:4 * i2 + 4, tt * 128:(tt + 1) * 128]
                            P.op('pe', ['KT%d_%d' % (i2, nk // 4)] + ['QT%d' % (4 * i2 + g) for g in range(4)], [pk],
                                 mm(pst[:].rearrange("p (g q) -> p g q", g=4), KT[hs, i2, nk * 128:(nk + 1) * 128],
                                    rhs, True, False))
                            P.op('pe', ['IDB', 'MB'], [pk], mm(pst[:], IDB[:], MB[:, ck, :], False, True))
                            P.op('act', [pk], ['E%d' % ebuf],
                                 lambda e, pst=pst, ebuf=ebuf: e.activation(out=EB[ebuf][:], in_=pst[:], func=AF.Exp))

                        def emit_pv(item, ebuf):
                            tt, n, i2, hh, ck, first, last = item
                            nk = n - 1 + ck
                            h = 2 * i2 + hh
                            hs = slice(64 * hh, 64 * hh + 64)
                            P.op('pe', ['V%d' % nk, 'E%d' % ebuf], ['PS5_%d' % hh],
                                 mm(PS5[hs, :], V[:, nk, h * 64:(h + 1) * 64], EB[ebuf][:], first, last))
                            P.op('pe', ['ONESB', 'E%d' % ebuf], ['PS6_%d' % hh],
                                 mm(PS6[hs, :], ONESB[:, 0:64], EB[ebuf][:], first, last))
                            if last and hh == 1:
                                P.op('dve', ['PS6_0', 'PS6_1', 'XS'], ['DS'],
                                     lambda e, i2=i2: e.tensor_tensor(out=DS[:], in0=PS6[:], in1=XS[:, i2, :], op=ALU.add))
                                P.op('dve', ['DS'], ['RD'], lambda e: e.reciprocal(out=RD[:], in_=DS[:]))
                                P.op('dve', ['PS5_0', 'PS5_1', 'RD'], ['OT%d' % i2],
                                     lambda e, i2=i2, tt=tt: e.tensor_tensor(
                                         out=OT[:, i2, :, tt * 128:(tt + 1) * 128],
                                         in0=PS5[:].rearrange("p (g q) -> p g q", g=4),
                                         in1=RD[:].rearrange("p (g q) -> p g q", g=4), op=ALU.mult))

                        for idx in range(len(items) + 1):
                            if idx < len(items):
                                emit_scores(items[idx], idx % 4, idx % 2)
                            if idx >= 1:
                                emit_pv(items[idx - 1], (idx - 1) % 4)
                        for m in range(8):
                            pk = 'PS%d' % (pq % 2)
                            pst = PS[pq % 2]
                            pq += 1
                            for j in range(8):
                                P.op('pe', ['WO%d' % j, 'OT%d' % (j // 4)], [pk],
                                     mm(pst[:], WO[:, j, m * 128:(m + 1) * 128], OT[:, j // 4, j % 4, :], j == 0, j == 7))
                            P.op('dve', [pk, 'ADA', 'XT'], ['XT'],
                                 lambda e, pst=pst, m=m, cs=cs: e.scalar_tensor_tensor(
                                     out=XT[:, m, cs], in0=pst[:], scalar=ada_vec(l, 2, s, m), in1=XT[:, m, cs],
                                     op0=ALU.mult, op1=ALU.add))
                    P.barrier()

            def emit_ssm(i, l, s):
                with ExitStack() as es3:
                    UT = es3.enter_context(sb("UT", [128, 8, T], BF16))
                    WB = [es3.enter_context(sb("WB%d" % b, [128, 8, 256], BF16)) for b in range(2)]
                    SWB = [es3.enter_context(sb("SWB%d" % b, [128, 4096], BF16)) for b in range(2)]
                    SWK = [es3.enter_context(sb("SWK%d" % b, [128, 4608], BF16)) for b in range(2)]
                    SR = [[es3.enter_context(sb("SR%d%d" % (u, b), [128, 256], F32)) for b in range(2)] for u in range(2)]
                    SI = [[es3.enter_context(sb("SI%d%d" % (u, b), [128, 256], F32)) for b in range(2)] for u in range(2)]
                    XE = [es3.enter_context(sb("XE%d" % b, [128, 4, 2, 256], BF16)) for b in range(2)]
                    YA = es3.enter_context(sb("YA", [128, T], BF16))
                    G2 = es3.enter_context(sb("G2", [128, 256], F32))
                    G3 = es3.enter_context(sb("G3", [128, 256], F32))
                    G4 = es3.enter_context(sb("G4", [128, 256], F32))
                    SG = es3.enter_context(sb("SG", [128, 512], F32))
                    YF = es3.enter_context(sb("YF", [128, 512], F32))
                    for b in range(2):
                        P.op('pool', [], ['XE%d_%d%s' % (b, q_, ri_) for q_ in range(4) for ri_ in 'ri'], lambda e, b=b: e.memset(XE[b][:], 0.0))
                    wcnt = 0
                    pq = 0
                    for m in range(8):
                        b = wcnt % 2
                        wcnt += 1
                        P.dma('pool', WB[b][:, :, 0:128], w_in[i, :, :, m * 128:(m + 1) * 128], [], ['WBa%d' % b])
                        for blk in range(4):
                            cs = slice(blk * 512, (blk + 1) * 512)
                            pi = pq % 2
                            pq += 1
                            for kc in range(8):
                                P.op('pe', ['WBa%d' % b, 'HT%d_%d' % (kc, blk)], ['PS%d' % pi],
                                     mm(PS[pi][:], WB[b][:, kc, 0:128], HT[:, kc, cs], kc == 0, kc == 7))
                            P.op('act', ['PS%d' % pi], ['UT%d' % m],
                                 lambda e, m=m, cs=cs, pi=pi: e.copy(out=UT[:, m, cs], in_=PS[pi][:]))
                    def load_swb(cc):
                        P.dma('sp', SWB[cc % 2][:], ssm_scr[i, cc, :, 0:4096], ['SCR%d_%d' % (i, cc)], ['SWB%d' % (cc % 2)])

                    def load_swk(cc):
                        P.dma('sp', SWK[cc % 2][:], ssm_scr[i, cc, :, 4096:8704], ['SCR%d_%d' % (i, cc)], ['SWK%d' % (cc % 2)])

                    def emit_sn(cc):
                        swk = 'SWB%d' % (cc % 2)
                        BT8 = SWB[cc % 2][:, 0:4096].rearrange("p (t c) -> p t c", t=8)
                        for q in range(4):
                            h2, w = q // 2, q % 2
                            hs = slice(64 * h2, 64 * h2 + 64)
                            u = q % 2
                            pi = q % 2
                            for part in range(2):
                                for jj in range(8):
                                    P.op('pe', [swk, 'UT%d' % cc], ['PS%d' % pi],
                                         mm(PS[pi][:, part * 256:part * 256 + 256],
                                            BT8[hs, 7 - jj, w * 256 + part * 128:w * 256 + part * 128 + 128],
                                            UT[hs, cc, jj:T:8], jj == 0, jj == 7))
                            P.op('act', ['PS%d' % pi], ['SR%d0t' % u, 'SR%d0h' % u],
                                 lambda e, u=u, pi=pi: e.copy(out=SR[u][0][:], in_=PS[pi][:, 0:256]))
                            P.op('act', ['PS%d' % pi], ['SI%d0t' % u, 'SI%d0h' % u],
                                 lambda e, u=u, pi=pi: e.copy(out=SI[u][0][:], in_=PS[pi][:, 256:512]))
                            if q % 2 == 1:
                                gens = [emit_scan(cc, q - 1), emit_scan(cc, q)]
                                alive = True
                                while alive:
                                    alive = False
                                    for g_ in gens:
                                        try:
                                            next(g_)
                                            alive = True
                                        except StopIteration:
                                            pass

                    def emit_scan(cc, q):
                        j = cc * 4 + q
                        u = q % 2
                        NCH = 256
                        a = 0
                        for k in range(8):
                            d = 1 << k
                            b2 = 1 - a
                            ra, ia, rb, ib = 'SR%d%d' % (u, a), 'SI%d%d' % (u, a), 'SR%d%d' % (u, b2), 'SI%d%d' % (u, b2)
                            akr = AKS[:, i, 0, k, j:j + 1]
                            aki = AKS[:, i, 1, k, j:j + 1]
                            akn = AKS[:, i, 2, k, j:j + 1]
                            Ra, Ia, Rb, Ib = SR[u][a], SI[u][a], SR[u][b2], SI[u][b2]

                            def stt(out, in0, sc, in1, rk, wk):
                                P.op('dve', rk, wk, lambda e: e.scalar_tensor_tensor(
                                    out=out, in0=in0, scalar=sc, in1=in1, op0=ALU.mult, op1=ALU.add))
                            stt(Rb[:, d:NCH], Ra[:, 0:NCH - d], akr, Ra[:, d:NCH], [ra + 't', ra + 'h', 'AKS'], [rb + 't'])
                            stt(Rb[:, d:NCH], Ia[:, 0:NCH - d], akn, Rb[:, d:NCH], [ia + 't', ia + 'h', rb + 't', 'AKS'], [rb + 't'])
                            stt(Ib[:, d:NCH], Ia[:, 0:NCH - d], akr, Ia[:, d:NCH], [ia + 't', ia + 'h', 'AKS'], [ib + 't'])
                            stt(Ib[:, d:NCH], Ra[:, 0:NCH - d], aki, Ib[:, d:NCH], [ra + 't', ra + 'h', ib + 't', 'AKS'], [ib + 't'])
                            P.op('pool', [ra + 't', ra + 'h'], [rb + 'h'], lambda e, Ra=Ra, Rb=Rb, d=d: e.tensor_copy(out=Rb[:, 0:d], in_=Ra[:, 0:d]))
                            P.op('act', [ia + 't', ia + 'h'], [ib + 'h'], lambda e, Ia=Ia, Ib=Ib, d=d: e.copy(out=Ib[:, 0:d], in_=Ia[:, 0:d]))
                            a = b2
                            yield
                        xe = XE[cc % 2]
                        xk = 'XE%d_%d' % (cc % 2, q)
                        P.op('act', ['SR%d%dt' % (u, a), 'SR%d%dh' % (u, a)], [xk + 'r'],
                             lambda e, xe=xe, q=q, u=u, a=a: e.copy(out=xe[:, q, 0, 1:NCH], in_=SR[u][a][:, 0:NCH - 1]))
                        P.op('pool', ['SI%d%dt' % (u, a), 'SI%d%dh' % (u, a)], [xk + 'i'],
                             lambda e, xe=xe, q=q, u=u, a=a: e.tensor_copy(out=xe[:, q, 1, 1:NCH], in_=SI[u][a][:, 0:NCH - 1]))

                    def emit_y(cc):
                        swk = 'SWK%d' % (cc % 2)
                        KT = SWK[cc % 2][:, 0:512].rearrange("p (t c) -> p t c", t=8)
                        CA = SWK[cc % 2][:, 512:4608].rearrange("p (r t q c) -> p r t q c", r=2, t=8, q=4)
                        xe = XE[cc % 2]
                        for t in range(8):
                            pi = 4 + (t % 4)
                            psy = PS[pi]
                            for h2 in range(2):
                                hs = slice(64 * h2, 64 * h2 + 64)
                                pk = 'PS%d_%d' % (pi, h2)
                                for jj in range(t + 1):
                                    P.op('pe', [swk, 'UT%d' % cc], [pk],
                                         mm(psy[hs, 0:256], KT[hs, t - jj, :], UT[hs, cc, jj:T:8], jj == 0, False))
                                for w in range(2):
                                    q = 2 * h2 + w
                                    xk = 'XE%d_%d' % (cc % 2, q)
                                    P.op('pe', [swk, xk + 'r'], [pk],
                                         mm(psy[hs, 0:256], CA[:, 0, t, q, :], xe[:, q, 0, :], False, False))
                                    P.op('pe', [swk, xk + 'i'], [pk],
                                         mm(psy[hs, 0:256], CA[:, 1, t, q, :], xe[:, q, 1, :], False, w == 1))
                            pks = ['PS%d_0' % pi, 'PS%d_1' % pi]
                            P.op('act', pks, ['G2'], lambda e, psy=psy: e.activation(out=G2[:], in_=psy[:, 0:256], func=AF.Square))
                            P.op('dve', ['G2'], ['G3'], lambda e: e.tensor_scalar(
                                out=G3[:], in0=G2[:], scalar1=0.044715, scalar2=1.0, op0=ALU.mult, op1=ALU.add))
                            P.op('dve', ['G3'] + pks, ['G4'], lambda e, psy=psy: e.tensor_tensor(
                                out=G4[:], in0=G3[:], in1=psy[:, 0:256], op=ALU.mult))
                            P.op('act', ['G4'], ['G2'], lambda e: e.activation(out=G2[:], in_=G4[:], func=AF.Sigmoid, scale=1.5957691216057308))
                            P.op('dve', ['G2'] + pks, ['YA'], lambda e, psy=psy, t=t: e.tensor_tensor(
                                out=YA[:, t:T:8], in0=G2[:], in1=psy[:, 0:256], op=ALU.mult))
                        P.op('act', ['YA'], ['UT%d' % cc], lambda e, cc=cc: e.copy(out=UT[:, cc, :], in_=YA[:]))

                    load_swb(0)
                    for cc in range(8):
                        if cc + 1 < 8:
                            load_swb(cc + 1)
                        load_swk(cc)
                        emit_sn(cc)
                        if cc >= 1:
                            emit_y(cc - 1)
                    emit_y(7)
                    for m in range(8):
                        b = wcnt % 2
                        wcnt += 1
                        P.dma('pool', WB[b][:, :, 0:128], w_glu[i, :, :, m * 128:(m + 1) * 128], [], ['WBa%d' % b])
                        P.dma('pool', WB[b][:, :, 128:256], w_glu[i, :, :, D + m * 128:D + (m + 1) * 128], [], ['WBb%d' % b])
                        for blk in range(4):
                            cs = slice(blk * 512, (blk + 1) * 512)
                            uk = ['UT%d' % kc for kc in range(8)]
                            pa, pb_ = (0, 1) if (blk % 2 == 0) else (2, 3)
                            PA, PB_ = PS[pa], PS[pb_]
                            for kc in range(8):
                                P.op('pe', ['WBa%d' % b] + uk, ['PS%d' % pa], mm(PA[:], WB[b][:, kc, 0:128], UT[:, kc, cs], kc == 0, kc == 7))
                            for kc in range(8):
                                P.op('pe', ['WBb%d' % b] + uk, ['PS%d' % pb_], mm(PB_[:], WB[b][:, kc, 128:256], UT[:, kc, cs], kc == 0, kc == 7))
                            P.op('act', ['PS%d' % pb_], ['SG'], lambda e, PB_=PB_: e.activation(out=SG[:], in_=PB_[:], func=AF.Sigmoid))
                            P.op('dve', ['PS%d' % pa, 'SG'], ['YF'], lambda e, PA=PA: e.tensor_tensor(out=YF[:], in0=PA[:], in1=SG[:], op=ALU.mult))
                            P.op('dve', ['YF', 'ADA', 'XT%d_%d' % (m, blk)], ['XT%d_%d' % (m, blk)],
                                 lambda e, m=m, cs=cs: e.scalar_tensor_tensor(
                                     out=XT[:, m, cs], in0=YF[:], scalar=ada_vec(l, 2, s, m), in1=XT[:, m, cs],
                                     op0=ALU.mult, op1=ALU.add))
                    P.barrier()

            def emit_moe(l, s):
                with ExitStack() as es:
                    WGU = [es.enter_context(sb("WGU%d" % b, [128, 8, 512], BF16)) for b in range(2)]
                    WDN = [es.enter_context(sb("WDN%d" % b, [128, 2, D], BF16)) for b in range(2)]
                    SEL = es.enter_context(sb("SEL", [32, 32, 128], F32))
                    GBS = [es.enter_context(sb("GBS%d" % b, [128, 512], BF16)) for b in range(2)]
                    TS = [es.enter_context(sb("TS%d" % b, [128, 2, 512], BF16)) for b in range(2)]
                    T2 = [es.enter_context(sb("T2%d" % b, [128, 2, 512], BF16)) for b in range(2)]
                    HID = [es.enter_context(sb("HID%d" % b, [128, 2, 512], BF16)) for b in range(2)]
                    P.dma('sp', SEL[:], sel[:, :, :], [], ['SEL'])

                    def load_w(e):
                        b = e % 2
                        for kc in range(8):
                            P.dma('pool', WGU[b][:, kc, :], wgu[l, e, :, kc, :], [], ['WGU%d' % b])
                        for f in range(2):
                            P.dma('pool', WDN[b][:, f, :], wdn[l, e, :, f, :], [], ['WDN%d' % b])
                    load_w(0)
                    items = [(e, blk) for e in range(NE) for blk in range(4)]
                    ycnt = [0]

                    def emit_gu(it, k):
                        e, blk = it
                        b = e % 2
                        r = k % 2
                        cs = slice(blk * 512, (blk + 1) * 512)
                        hkeys = ['HT%d_%d' % (kc, blk) for kc in range(8)]
                        P.op('pe', ['SEL', 'GT%d' % blk], ['PS4'], mm(PS4[:], SEL[:, e, :], GT[:, cs], True, True))
                        P.op('act', ['PS4'], ['GBS%d' % r], lambda en: en.copy(out=GBS[r][:], in_=PS4[:]))
                        for f in range(2):
                            for kc in range(8):
                                P.op('pe', ['WGU%d' % b] + hkeys, ['PS%d' % f],
                                     mm(PS[f][:], WGU[b][:, kc, f * 128:(f + 1) * 128], HT[:, kc, cs], kc == 0, kc == 7))
                            for kc in range(8):
                                P.op('pe', ['WGU%d' % b] + hkeys, ['PS%d' % (2 + f)],
                                     mm(PS[2 + f][:], WGU[b][:, kc, 256 + f * 128:256 + (f + 1) * 128], HT[:, kc, cs],
                                        kc == 0, kc == 7))
                            P.op('act', ['PS%d' % f], ['TS%d_%d' % (r, f)],
                                 lambda en, f=f: en.activation(out=TS[r][:, f, :], in_=PS[f][:], func=AF.Silu))
                            P.op('pool', ['TS%d_%d' % (r, f), 'GBS%d' % r], ['T2%d_%d' % (r, f)],
                                 lambda en, f=f: en.tensor_tensor(out=T2[r][:, f, :], in0=TS[r][:, f, :],
                                                                  in1=GBS[r][:], op=ALU.mult))
                            P.op('dve', ['PS%d' % (2 + f), 'T2%d_%d' % (r, f)], ['HID%d_%d' % (r, f)],
                                 lambda en, f=f: en.tensor_tensor(out=HID[r][:, f, :], in0=PS[2 + f][:],
                                                                  in1=T2[r][:, f, :], op=ALU.mult))

                    def emit_y(it, k):
                        e, blk = it
                        b = e % 2
                        r = k % 2
                        cs = slice(blk * 512, (blk + 1) * 512)
                        for m in range(8):
                            pi = 5 + (ycnt[0] % 3)
                            ycnt[0] += 1
                            for f in range(2):
                                P.op('pe', ['WDN%d' % b, 'HID%d_%d' % (r, f)], ['PS%d' % pi],
                                     mm(PS[pi][:], WDN[b][:, f, m * 128:(m + 1) * 128], HID[r][:, f, :], f == 0, f == 1))
                            P.op('dve', ['PS%d' % pi, 'ADA', 'XT%d_%d' % (m, blk)], ['XT%d_%d' % (m, blk)],
                                 lambda en, m=m, pi=pi: en.scalar_tensor_tensor(
                                     out=XT[:, m, cs], in0=PS[pi][:], scalar=ada_vec(l, 5, s, m), in1=XT[:, m, cs],
                                     op0=ALU.mult, op1=ALU.add))

                    for k in range(len(items) + 1):
                        if k < len(items):
                            emit_gu(items[k], k)
                        if k >= 1:
                            emit_y(items[k - 1], k - 1)
                        if k < len(items) and items[k][1] == 0 and items[k][0] + 1 < NE:
                            load_w(items[k][0] + 1)
                    P.barrier()

            def emit_moe_sparse(l, s):
                NB = 64
                with ExitStack() as es:
                    WGU = [es.enter_context(sb("WGU%d" % b, [128, 8, 512], BF16)) for b in range(3)]
                    WDN = [es.enter_context(sb("WDN%d" % b, [128, 2, D], BF16)) for b in range(3)]
                    XB = [es.enter_context(sb("XB%d" % b, [128, 1024], BF16)) for b in range(2)]
                    XBT = [es.enter_context(sb("XBT%d" % b, [128, 8, 128], BF16)) for b in range(2)]
                    TS = [es.enter_context(sb("TS%d" % b, [128, 256], BF16)) for b in range(2)]
                    HID = [es.enter_context(sb("HID%d" % b, [128, 256], BF16)) for b in range(2)]
                    YR = [es.enter_context(sb("YR%d" % b, [128, 1024], F32)) for b in range(2)]
                    YTOK = [es.enter_context(sb("YTOK%d" % b, [128, 1024], F32)) for b in range(4)]
                    YTK2 = [es.enter_context(sb("YTK2%d" % b, [128, 1024], F32)) for b in range(4)]
                    PSB = PS7[:].bitcast(BF16)
                    IDXGF = IDXG[:].bitcast(F32)
                    if 'yd' not in _regs:
                        _regs['yd'] = nc.gpsimd.alloc_register(name="ydmax_reg")
                        nc.gpsimd.reg_mov(_regs['yd'], 8191)
                    if 'hd' not in _regs:
                        _regs['hd'] = nc.gpsimd.alloc_register(name="hdmax_reg")
                        nc.gpsimd.reg_mov(_regs['hd'], 2175)
                    if 'wmax' not in _regs:
                        _regs['wmax'] = nc.gpsimd.alloc_register(name="wmax_reg")
                        nc.gpsimd.reg_mov(_regs['wmax'], L * NE * 128 - 1)
                    WMAX = _regs['wmax']

                    def fetch_x(i, b):
                        r = i % 2
                        P.idma('pool', lambda g: g.indirect_dma_start(
                            out=XB[r][:], out_offset=None, in_=HD[:, :],
                            in_offset=bass.IndirectOffsetOnAxis(ap=IDXG[:, b, 0:1], axis=0),
                            bounds_check=_regs['hd'], oob_is_err=False), ['IDXG', 'HD'], ['XB%d' % r])

                    def fetch(i, b):
                        w3 = i % 3
                        for (dst, srcw, hk) in ((WGU[w3][:, 0:4, :].rearrange("p a c -> p (a c)"), wguA, 'a'),
                                                (WGU[w3][:, 4:8, :].rearrange("p a c -> p (a c)"), wguB, 'b')):
                            P.idma('pool', lambda g, dst=dst, srcw=srcw: g.indirect_dma_start(
                                out=dst, out_offset=None, in_=srcw[:, :],
                                in_offset=bass.IndirectOffsetOnAxis(ap=IDXW[:, b:b + 1], axis=0),
                                bounds_check=WMAX, oob_is_err=False), ['IDXW'], ['WGU%s%d' % (hk, w3)])
                        P.idma('pool', lambda g: g.indirect_dma_start(
                            out=WDN[w3][:].rearrange("p a c -> p (a c)"), out_offset=None, in_=wdn2[:, :],
                            in_offset=bass.IndirectOffsetOnAxis(ap=IDXW[:, b:b + 1], axis=0),
                            bounds_check=WMAX, oob_is_err=False), ['IDXW'], ['WDN%d' % w3])

                    def emit_gu(i, b):
                        r = i % 2
                        w3 = i % 3
                        for kc in range(8):
                            P.op('pe', ['XB%d' % r, 'IDB'], ['PS7'],
                                 lambda pe, kc=kc: pe.transpose(PSB[:, kc * 128:(kc + 1) * 128], XB[r][:, kc * 128:(kc + 1) * 128], IDB[:]))
                        P.op('act', ['PS7'], ['XBT%d' % r], lambda e: e.copy(out=XBT[r][:].rearrange("p a c -> p (a c)"), in_=PSB))
                        pk = 'PS%d' % r
                        for grp in range(4):
                            for kc in range(8):
                                P.op('pe', ['WGUa%d' % w3, 'WGUb%d' % w3, 'XBT%d' % r], [pk],
                                     mm(PS[r][:, grp * 128:(grp + 1) * 128], WGU[w3][:, kc, grp * 128:(grp + 1) * 128],
                                        XBT[r][:, kc, :], kc == 0, kc == 7))
                        P.op('act', [pk], ['TS%d' % r], lambda e: e.activation(out=TS[r][:], in_=PS[r][:, 0:256], func=AF.Silu))
                        P.op('dve', [pk, 'TS%d' % r], ['HID%d' % r], lambda e: e.tensor_tensor(
                            out=HID[r][:], in0=PS[r][:, 256:512], in1=TS[r][:], op=ALU.mult))

                    def emit_y(i, b):
                        r = i % 2
                        w3 = i % 3
                        for hf in range(2):
                            pi = 2 + 2 * r + hf
                            for f in range(2):
                                P.op('pe', ['WDN%d' % w3, 'HID%d' % r], ['PS%d' % pi],
                                     mm(PS[pi][:], HID[r][:, f * 128:(f + 1) * 128], WDN[w3][:, f, hf * 512:(hf + 1) * 512], f == 0, f == 1))
                            if hf == 0:
                                P.op('act', ['PS%d' % pi, 'IDXG'], ['YR%d_0' % r], lambda e, pi=pi: e.activation(
                                    out=YR[r][:, 0:512], in_=PS[pi][:], func=AF.Identity, scale=IDXGF[:, b, 1:2]))
                            else:
                                P.op('dve', ['PS%d' % pi, 'IDXG'], ['YR%d_1' % r], lambda e, pi=pi: e.tensor_scalar(
                                    out=YR[r][:, 512:1024], in0=PS[pi][:], scalar1=IDXGF[:, b, 1:2], scalar2=None, op0=ALU.mult))
                        P.dma('sp', YD[b * 128:(b + 1) * 128, :], YR[r][:], ['YR%d_0' % r, 'YR%d_1' % r], ['YD%d' % b])

                    seq = []
                    for k in range(MOE_R1):
                        seq += [k, MOE_R0 + k, MOE_R0 + MOE_R1 + k]
                    seq += list(range(MOE_R1, MOE_R0))
                    assert sorted(seq) == list(range(NB)) and MOE_R0 - MOE_R1 <= 1 and NB - MOE_R0 - MOE_R1 == MOE_R1
                    pos = list(enumerate(seq))
                    fetch_x(*pos[0])
                    fetch_x(*pos[1])
                    fetch(*pos[0])
                    fetch(*pos[1])
                    fetch(*pos[2])
                    for j in range(NB + 1):
                        if j < NB:
                            emit_gu(*pos[j])
                            if j + 2 < NB:
                                fetch_x(*pos[j + 2])
                        if j >= 1:
                            emit_y(*pos[j - 1])
                            if j + 2 < NB:
                                fetch(*pos[j + 2])
                    cnt = 0
                    for n in range(16):
                        yb = n % 4
                        ydk = ['YD%d' % b_ for b_ in range(NB)]
                        for (dstt, kk_, nm) in ((YTOK[yb], 0, ['YTOKa%d' % yb, 'YTOK%d' % yb]), (YTK2[yb], 1, ['YTOKb%d' % yb])):
                            P.idma('pool', lambda g, dstt=dstt, kk_=kk_: g.indirect_dma_start(
                                out=dstt[:], out_offset=None, in_=YD[:, :],
                                in_offset=bass.IndirectOffsetOnAxis(ap=DSTI[:, 2 * n + kk_:2 * n + kk_ + 1], axis=0),
                                bounds_check=_regs['yd'], oob_is_err=False), ydk + ['DSTI'], nm)
                        P.op('dve', ['YTOKa%d' % yb, 'YTOKb%d' % yb, 'YTOK%d' % yb], ['YTOK%d' % yb], lambda e, yb=yb: e.tensor_tensor(
                            out=YTOK[yb][:], in0=YTOK[yb][:], in1=YTK2[yb][:], op=ALU.add))
                        for m in range(8):
                            pi = cnt % 4
                            cnt += 1
                            P.op('pe', ['YTOK%d' % yb, 'IDF'], ['PS%d' % pi],
                                 lambda pe, m=m, pi=pi, yb=yb: pe.transpose(PS[pi][:, 0:128], YTOK[yb][:, m * 128:(m + 1) * 128], IDF[:]))
                            P.op('dve', ['PS%d' % pi, 'ADA', 'XT%d' % m], ['XT%d' % m],
                                 lambda e, m=m, pi=pi, n=n: e.scalar_tensor_tensor(
                                     out=XT[:, m, n * 128:(n + 1) * 128], in0=PS[pi][:, 0:128], scalar=ada_vec(l, 5, s, m),
                                     in1=XT[:, m, n * 128:(n + 1) * 128], op0=ALU.mult, op1=ALU.add))
                    P.barrier()

            if SPARSE:
                with ExitStack() as esz:
                    ZB = esz.enter_context(sb("ZB", [128, 1024], BF16))
                    P.op('dve', [], ['ZB'], lambda e: e.memset(ZB[:], 0.0))
                    P.dma('sp', HD[2048:2176, :], ZB[:], ['ZB'], ['HDz'])
                    P.barrier()
            for s in range(n_seq):
                for kc in range(8):
                    P.dma('sp', XT[:, kc, :], xT[s, kc * 128:(kc + 1) * 128, :], [], ['XTL%d' % kc])
                for l in layers:
                    emit_norm(l, s, 0)
                    if l % 2 == 0:
                        emit_attn(l // 2, l, s)
                    else:
                        emit_ssm(l // 2, l, s)
                    if dbg != 'mix':
                        with ExitStack() as esg:
                            if SPARSE:
                                IDXG = esg.enter_context(sb("IDXG", [128, 64, 8], I32))
                                IDXW = esg.enter_context(sb("IDXW", [128, 64], I32))
                                DSTI = esg.enter_context(sb("DSTI", [128, 32], I32))
                                emit_norm(l, s, 1)
                                emit_moe_sparse(l, s)
                            else:
                                GT = esg.enter_context(sb("GT", [32, T], F32))
                                emit_norm(l, s, 1)
                                emit_moe(l, s)
                    P.barrier()
                for kc in range(8):
                    P.dma('sp', yT[s, kc * 128:(kc + 1) * 128, :], XT[:, kc, :], ['XTL%d' % kc], ['yT%d' % kc])
            P.barrier()
    return nc


def _pmajor(w, kc):
    sh = w.shape
    w = w.reshape(sh[:-2] + (kc, 128, sh[-1]))
    nd = w.ndim
    perm = list(range(nd - 3)) + [nd - 2, nd - 3, nd - 1]
    return np.ascontiguousarray(w.transpose(perm))


def prep_shared(inp):
    f = np.float32
    sh = {}
    sh['gmix'] = np.ascontiguousarray(inp['norm_mix'].reshape(L, 8, 128).transpose(2, 0, 1)).astype(f)
    sh['gffn'] = np.ascontiguousarray(inp['norm_ffn'].reshape(L, 8, 128).transpose(2, 0, 1)).astype(f)
    sh['w_ada'] = _pmajor(inp['w_ada'], 8)
    sh['b_ada'] = np.ascontiguousarray(inp['b_ada'].reshape(L, 48, 128).transpose(2, 0, 1))
    cols = []
    for c in range(8):
        for s2 in range(2):
            hd = 4 * (2 * (c // 4) + s2) + (c % 4)
            cols += list(range(hd * 64, hd * 64 + 64))
    cols += list(range(1024, 1536))
    sh['wqkv'] = _pmajor(inp['attn_w_qkv'][:, :, cols], 8)
    sh['qgain'] = np.ascontiguousarray(np.tile(inp['attn_q_gain'], (1, 2)).T)
    sh['kgain'] = np.ascontiguousarray(np.tile(inp['attn_k_gain'], (1, 2)).T)
    sk = np.zeros((2, 128, 2, 512), f)
    for i2 in range(2):
        for hh in range(2):
            h = 2 * i2 + hh
            for g in range(4):
                sk[:, 64 * hh:64 * hh + 64, i2, g * 128:(g + 1) * 128] = inp['attn_sinks'][:, 4 * h + g][:, None, None]
    sh['sinks'] = sk
    rows = []
    for i2 in range(2):
        for g in range(4):
            for hh in range(2):
                hd = 4 * (2 * i2 + hh) + g
                rows += list(range(hd * 64, hd * 64 + 64))
    sh['wo'] = _pmajor(inp['attn_w_o'][:, rows, :], 8)
    mb = np.zeros((128, 2, 4, 128), f)
    sidx = np.arange(128)[:, None]
    qidx = np.arange(128)[None, :]
    mb[:, 0] = np.where(sidx > qidx, 0.0, NEG)[:, None, :]
    mb[:, 1] = np.where(sidx <= qidx, 0.0, NEG)[:, None, :]
    sh['maskb'] = mb.reshape(128, 2, 512).astype(ml_dtypes.bfloat16)
    sh['identb'] = np.eye(128, dtype=f).astype(ml_dtypes.bfloat16)
    ob = np.zeros((128, 128), f)
    ob[:64, :64] = 1
    ob[64:, 64:] = 1
    sh['onesblk'] = ob.astype(ml_dtypes.bfloat16)
    sh['identf'] = np.eye(128, dtype=f)
    sh['ident2'] = np.concatenate([np.eye(64, dtype=f), np.eye(64, dtype=f)], axis=0)
    sh['wr'] = _pmajor(np.concatenate([inp['moe_w_group'], inp['moe_w_expert']], axis=-1), 8)
    sh['br'] = np.ascontiguousarray(np.concatenate([inp['moe_b_group'], inp['moe_b_expert']], axis=-1)[None])
    wgu_p = _pmajor(np.concatenate([inp['moe_w_gate'], inp['moe_w_up']], axis=-1), 8).reshape(L * NE * 128, 4096)
    sh['wguA'] = np.ascontiguousarray(wgu_p[:, 0:2048])
    sh['wguB'] = np.ascontiguousarray(wgu_p[:, 2048:4096])
    del wgu_p
    sh['wdn2'] = _pmajor(inp['moe_w_down'], 2).reshape(L * NE * 128, 2048)
    kk = np.arange(128)
    sh['trib'] = (kk[:, None] < kk[None, :]).astype(f).astype(ml_dtypes.bfloat16)
    sh['toki'] = (np.arange(16)[None, :] * 128 + kk[:, None]).astype(np.int32)
    ri = np.zeros((128, 64, 8), np.int32)
    ri[:, :, 0] = 2048 + kk[:, None]
    sh['rinit'] = ri.reshape(128, 512)
    sh['b128'] = np.broadcast_to((np.arange(64) * 128).astype(f)[None, :], (128, 64)).copy()
    sh['pidx'] = kk.astype(f)[:, None].copy()
    sh['w_in'] = _pmajor(inp['ssm_w_in'], 8)
    sh['w_glu'] = _pmajor(inp['ssm_w_glu'], 8)
    sh['ssm_d'] = np.ascontiguousarray(inp['ssm_d'].reshape(2, 8, 128).transpose(2, 0, 1))

    def gp(a):
        return np.ascontiguousarray(a.reshape(2, 32, 2, 64).transpose(2, 3, 0, 1).reshape(128, 2, 32))
    sh['lam_re'] = gp(inp['ssm_lam_re'])
    sh['lam_im'] = gp(inp['ssm_lam_im'])
    sh['log_dt'] = gp(np.broadcast_to(inp['ssm_log_dt'][:, :, None], (2, 64, 64)))

    def blk(a):
        o = np.zeros((128, 2, 32, 64), f)
        a5 = a.reshape(2, 32, 2, 64, 16)
        for g2 in range(2):
            for w in range(2):
                o[64 * g2:64 * g2 + 64, :, w::2, 32 * w + 16 * g2:32 * w + 16 * g2 + 16] = \
                    a5[:, w::2, g2].transpose(2, 0, 1, 3)
        return o
    sh['b_re'] = blk(inp['ssm_b_re'])
    sh['b_im'] = blk(inp['ssm_b_im'])
    sh['c_re'] = blk(inp['ssm_c_re'].transpose(0, 1, 3, 2))
    sh['c_im'] = blk(inp['ssm_c_im'].transpose(0, 1, 3, 2))
    return {k: np.ascontiguousarray(v) for k, v in sh.items()}


def prep_core(inp, core):
    b0 = core * SEQ_PER_CORE
    x = inp['x'][b0:b0 + SEQ_PER_CORE]
    xTn = np.ascontiguousarray(x.transpose(0, 2, 1))
    c = inp['c'][b0:b0 + SEQ_PER_CORE]
    cTn = np.ascontiguousarray(c.reshape(SEQ_PER_CORE, 8, 128).transpose(2, 1, 0))
    return {'xT': xTn, 'cT': cTn}


_CACHE = {}


def kernel(**inputs):
    inp = {k: np.asarray(v) for k, v in inputs.items()}
    if 'nc' not in _CACHE:
        _CACHE['nc'] = build_program()
    nc = _CACHE['nc']
    shared = prep_shared(inp)
    in_maps = []
    for core in range(NCORES):
        m = dict(shared)
        m.update(prep_core(inp, core))
        in_maps.append(m)
    res = run_bass_kernel_spmd(nc, in_maps, core_ids=list(range(NCORES)))
    out = np.empty((NCORES * SEQ_PER_CORE, T, D), np.float32)
    for core in range(NCORES):
        yT = res.results[core]['yT']
        for s in range(SEQ_PER_CORE):
            out[core * SEQ_PER_CORE + s] = yT[s].T
    return out
```

```python
import numpy as np
from contextlib import ExitStack
import ml_dtypes
import concourse.bass as bass
import concourse.mybir as mybir
from concourse.bass_utils import run_bass_kernel_spmd

F32 = mybir.dt.float32
BF16 = mybir.dt.bfloat16
I32 = mybir.dt.int32
SPARSE = True
MOE_R0, MOE_R1 = 22, 21
AF = mybir.ActivationFunctionType
ALU = mybir.AluOpType
AX = mybir.AxisListType

D = 1024
T = 2048
L = 4
NE = 32
EPS = 1e-6
NCORES = 8
SEQ_PER_CORE = 2
NEG = -30000.0
NDS = 56


class Prog:
    def __init__(self, nc):
        self.nc = nc
        self.eng = {'pe': nc.tensor, 'act': nc.scalar, 'dve': nc.vector,
                    'pool': nc.gpsimd, 'sp': nc.sync}
        self.sems = {}
        for e in self.eng:
            self.sems['E' + e] = nc.alloc_semaphore(name='sem_' + e)
        self.ecnt = {e: 0 for e in self.eng}
        self.waited = {e: {} for e in self.eng}
        self.lw = {}
        self.rd = {}
        self.dval = [0] * NDS
        for i in range(NDS):
            self.sems['D%d' % i] = nc.alloc_semaphore(name='dsem%d' % i)
        self.dnext = {'sp': 0, 'pool': 0, 'act': 0}
        self.drange = {'sp': (0, NDS // 2), 'act': (0, NDS // 2), 'pool': (NDS // 2, NDS)}
        self.nwaits = 0

    def _deps(self, reads, writes):
        deps = {}

        def add(ev):
            if ev is None:
                return
            k, v = ev
            if deps.get(k, 0) < v:
                deps[k] = v
        for k in reads:
            add(self.lw.get(k))
        for k in writes:
            add(self.lw.get(k))
            for ev in self.rd.get(k, {}).items():
                add(ev)
        return deps

    def _wait(self, e, deps):
        w = self.waited[e]
        for k, v in deps.items():
            if w.get(k, 0) < v:
                self.eng[e].wait_ge(self.sems[k], v)
                w[k] = v
                self.nwaits += 1

    def _record(self, reads, writes, ev):
        k0, v0 = ev
        for k in writes:
            self.lw[k] = ev
            self.rd[k] = {}
        for k in reads:
            d = self.rd.setdefault(k, {})
            if d.get(k0, 0) < v0:
                d[k0] = v0

    def op(self, e, reads, writes, fn):
        deps = self._deps(reads, writes)
        if e == 'pe':
            deps.pop('Epe', None)
        self._wait(e, deps)
        inst = fn(self.eng[e])
        self.ecnt[e] += 1
        inst.then_inc(self.sems['E' + e], 1)
        self._record(reads, writes, ('E' + e, self.ecnt[e]))

    def dma(self, q, out, in_, reads, writes, **kw):
        deps = self._deps(reads, writes)
        lo, hi = self.drange[q]
        i = lo + self.dnext[q]
        self.dnext[q] = (self.dnext[q] + 1) % (hi - lo)
        if self.dval[i] > 0:
            k = 'D%d' % i
            if deps.get(k, 0) < self.dval[i]:
                deps[k] = self.dval[i]
        self._wait(q, deps)
        self.dval[i] += 16
        self.eng[q].dma_start(out=out, in_=in_, **kw).then_inc(self.sems['D%d' % i], 16)
        self._record(reads, writes, ('D%d' % i, self.dval[i]))

    def idma(self, q, fn, reads, writes):
        deps = self._deps(reads, writes)
        lo, hi = self.drange[q]
        i = lo + self.dnext[q]
        self.dnext[q] = (self.dnext[q] + 1) % (hi - lo)
        if self.dval[i] > 0:
            k = 'D%d' % i
            if deps.get(k, 0) < self.dval[i]:
                deps[k] = self.dval[i]
        self._wait(q, deps)
        self.dval[i] += 16
        fn(self.eng[q]).then_inc(self.sems['D%d' % i], 16)
        self._record(reads, writes, ('D%d' % i, self.dval[i]))

    def barrier(self):
        deps = {}
        for e in self.eng:
            if self.ecnt[e] > 0:
                deps['E' + e] = self.ecnt[e]
        for i in range(NDS):
            if self.dval[i] > 0:
                deps['D%d' % i] = self.dval[i]
        for e in self.eng:
            d = dict(deps)
            self._wait(e, d)
        self.lw = {}
        self.rd = {}


def mm(ps, lhsT, rhs, start, stop):
    return lambda pe: pe.matmul(ps, lhsT, rhs, start=start, stop=stop)


def build_program(n_layers=L, n_seq=SEQ_PER_CORE, dbg=None, layers=None):
    layers = list(range(n_layers)) if layers is None else layers
    n_layers = L
    nc = bass.Bass("TRN2", target_bir_lowering=False)
    P = Prog(nc)

    def din(name, shape, dt=F32):
        return nc.dram_tensor(name, list(shape), dt, kind="ExternalInput").ap()

    xT = din("xT", [SEQ_PER_CORE, D, T])
    cT = din("cT", [128, 8, 2])
    gmix = din("gmix", [128, L, 8])
    gffn = din("gffn", [128, L, 8])
    w_ada = din("w_ada", [L, 128, 8, 6 * D])
    b_ada = din("b_ada", [128, L, 48])
    wqkv = din("wqkv", [2, 128, 8, 1536])
    qgain = din("qgain", [128, 2])
    kgain = din("kgain", [128, 2])
    sinks = din("sinks", [2, 128, 2, 512])
    wo = din("wo", [2, 128, 8, D])
    maskb = din("maskb", [128, 2, 512], BF16)
    identb = din("identb", [128, 128], BF16)
    onesblk = din("onesblk", [128, 128], BF16)
    wr = din("wr", [L, 128, 8, 36])
    br = din("br", [1, L, 36])
    if not SPARSE:
        wgu = din("wgu", [L, NE, 128, 8, 512])
        wdn = din("wdn", [L, NE, 128, 2, D])
        sel = din("sel", [32, 32, 128])
    identf = din("identf", [128, 128])
    w_in = din("w_in", [2, 128, 8, D])
    w_glu = din("w_glu", [2, 128, 8, 2 * D])
    ssm_d = din("ssm_d", [128, 2, 8])
    lam_re = din("lam_re", [128, 2, 32])
    lam_im = din("lam_im", [128, 2, 32])
    log_dt = din("log_dt", [128, 2, 32])
    b_re = din("b_re", [128, 2, 32, 64])
    b_im = din("b_im", [128, 2, 32, 64])
    c_re = din("c_re", [128, 2, 32, 64])
    c_im = din("c_im", [128, 2, 32, 64])
    ident2 = din("ident2", [128, 64])
    wguA = din("wguA", [L * NE * 128, 2048])
    wguB = din("wguB", [L * NE * 128, 2048])
    wdn2 = din("wdn2", [L * NE * 128, 2048])
    trib = din("trib", [128, 128], BF16)
    toki = din("toki", [128, 16], I32)
    rinit = din("rinit", [128, 512], I32)
    b128 = din("b128", [128, 64])
    pidx = din("pidx", [128, 1])
    HD = nc.dram_tensor("HD", [2176, 1024], BF16, kind="Internal").ap()
    YD = nc.dram_tensor("YD", [8192, 1024], F32, kind="Internal").ap()
    ROWT = nc.dram_tensor("ROWT", [8192, 8], I32, kind="Internal").ap()
    yT = nc.dram_tensor("yT", [SEQ_PER_CORE, D, T], F32, kind="ExternalOutput").ap()
    ssm_scr = nc.dram_tensor("ssm_scr", [2, 8, 128, 8704], BF16, kind="Internal").ap()

    _uid = [0]
    _regs = {}

    def sb(name, shape, dt):
        _uid[0] += 1
        return nc.sbuf_tensor("%s_%d" % (name, _uid[0]), shape, dt)
    ps_ = nc.psum_tensor

    with ExitStack() as es:
        AKS = es.enter_context(sb("AKS", [128, 2, 3, 8, 32], F32))
        ADA = es.enter_context(sb("ADA", [128, L, 48, 2], F32))
        SCL = es.enter_context(sb("SCL", [128, L, 2, 2, 8], F32))
        ONESB = es.enter_context(sb("ONESB", [128, 128], BF16))
        ONEBLK = es.enter_context(sb("ONEBLK", [128, 128], BF16))
        IDB = es.enter_context(sb("IDB", [128, 128], BF16))
        TRIB = es.enter_context(sb("TRIB", [128, 128], BF16))
        IDF = es.enter_context(sb("IDF", [128, 128], F32))
        ONEF = es.enter_context(sb("ONEF", [1, 128], F32))
        GMIX = es.enter_context(sb("GMIX", [128, L, 8], F32))
        GFFN = es.enter_context(sb("GFFN", [128, L, 8], F32))
        BR = es.enter_context(sb("BR", [1, L, 36], F32))
        PS0 = es.enter_context(ps_("PS0", [128, 512], F32))
        PS1 = es.enter_context(ps_("PS1", [128, 512], F32))
        PS2 = es.enter_context(ps_("PS2", [128, 512], F32))
        PS3 = es.enter_context(ps_("PS3", [128, 512], F32))
        PS4 = es.enter_context(ps_("PS4", [128, 512], F32))
        PS5 = es.enter_context(ps_("PS5", [128, 512], F32))
        PS6 = es.enter_context(ps_("PS6", [128, 512], F32))
        PS7 = es.enter_context(ps_("PS7", [128, 512], F32))
        PS = [PS0, PS1, PS2, PS3, PS4, PS5, PS6, PS7]

        P.op('dve', [], ['ONESB'], lambda e: e.memset(ONESB[:], 1.0))
        P.op('dve', [], ['ONEF'], lambda e: e.memset(ONEF[:], 1.0))
        P.dma('sp', ONEBLK[:], onesblk[:, :], [], ['ONEBLK'])
        P.dma('sp', IDB[:], identb[:, :], [], ['IDB'])
        P.dma('sp', TRIB[:], trib[:, :], [], ['TRIB'])
        P.dma('sp', IDF[:], identf[:, :], [], ['IDF'])
        P.dma('sp', GMIX[:], gmix[:, :, :], [], ['GMIX'])
        P.dma('sp', GFFN[:], gffn[:, :, :], [], ['GFFN'])
        P.dma('sp', BR[:], br[:, :, :], [], ['BR'])

        def ada_phase(prep_gens):
            with ExitStack() as es:
                CT = es.enter_context(sb("CT", [128, 8, 2], F32))
                CA = es.enter_context(sb("CA", [128, 8, 2], F32))
                BADA = es.enter_context(sb("BADA", [128, L, 48], F32))
                WA0 = es.enter_context(sb("WA0", [128, 8, 768], F32))
                WA1 = es.enter_context(sb("WA1", [128, 8, 768], F32))
                WA = [WA0, WA1]
                P.dma('sp', CT[:], cT[:, :, :], [], ['CT'])
                P.dma('sp', BADA[:], b_ada[:, :, :], [], ['BADA'])
                P.op('act', ['CT'], ['CA'], lambda e: e.activation(out=CA[:], in_=CT[:], func=AF.Silu))

                def ada_block(it):
                    l, cb = it // 8, it % 8
                    W = WA[it % 2]
                    wk = 'WA%d' % (it % 2)
                    pk = 'PS%d' % (it % 2)
                    pst = PS[it % 2]
                    P.dma('sp', W[:], w_ada[l, :, :, cb * 768:(cb + 1) * 768], [], [wk])
                    for j in range(6):
                        for kc in range(8):
                            P.op('pe', [wk, 'CA'], [pk],
                                 mm(pst[:, 2 * j:2 * j + 2], W[:, kc, j * 128:(j + 1) * 128],
                                    CA[:, kc, :], kc == 0, kc == 7))
                    pv = pst[:, 0:12].rearrange("p (j s) -> p j s", s=2)
                    bb = BADA[:, l, cb * 6:(cb + 1) * 6].unsqueeze(2).to_broadcast([128, 6, 2])
                    P.op('dve', [pk, 'BADA'], ['ADA%d_%d' % (l, cb)],
                         lambda e: e.tensor_tensor(out=ADA[:, l, cb * 6:(cb + 1) * 6, :], in0=pv, in1=bb, op=ALU.add))
                nblk = n_layers * 8
                pos = 0
                for g in prep_gens:
                    for _ in g:
                        for _k in range(2):
                            if pos < nblk:
                                ada_block(pos)
                                pos += 1
                while pos < nblk:
                    ada_block(pos)
                    pos += 1
                allk = ['ADA%d_%d' % (l, cb) for l in range(n_layers) for cb in range(8)]
                for l in range(n_layers):
                    for w in range(2):
                        G = GMIX if w == 0 else GFFN
                        off = 8 if w == 0 else 32
                        for s in range(2):
                            P.op('dve', allk + ['GMIX', 'GFFN'], ['SCL'],
                                 lambda e, l=l, w=w, s=s, G=G, off=off: e.scalar_tensor_tensor(
                                     out=SCL[:, l, w, s, :], in0=ADA[:, l, off:off + 8, s], scalar=1.0,
                                     in1=G[:, l, :], op0=ALU.add, op1=ALU.mult))
                P.barrier()

        def emit_ssm_prep(i):
            TWO_PI = 6.283185307179586
            with ExitStack() as es2:
                def t32(nm):
                    return es2.enter_context(sb(nm, [128, 32], F32))
                LR, LI, LDT, DTt, LRD, LID, MAG = [t32(n) for n in ("LR", "LI", "LDT", "DTt", "LRD", "LID", "MAG")]
                U1, KF, FR, NEGm, SINV, COSV = [t32(n) for n in ("U1", "KF", "FR", "NEGm", "SINV", "COSV")]
                ABR, ABI, DEN, RDN, NRE, A1, A2, FRE, FIM = [t32(n) for n in ("ABR", "ABI", "DEN", "RDN", "NRE", "A1", "A2", "FRE", "FIM")]
                KI = es2.enter_context(sb("KI", [128, 32], mybir.dt.int32))
                NPI = es2.enter_context(sb("NPI", [128, 1], F32))
                ID2 = es2.enter_context(sb("ID2", [128, 64], F32))
                DV = es2.enter_context(sb("DV", [128, 2, 8], F32))
                PWR = es2.enter_context(sb("PWR", [128, 9, 32], F32))
                PWI = es2.enter_context(sb("PWI", [128, 9, 32], F32))
                CRt = es2.enter_context(sb("CRt", [128, 32, 64], F32))
                CIt = es2.enter_context(sb("CIt", [128, 32, 64], F32))
                BBR = es2.enter_context(sb("BBR", [128, 32, 64], F32))
                BBI = es2.enter_context(sb("BBI", [128, 32, 64], F32))
                esh = ExitStack()
                BRt = esh.enter_context(sb("BRt", [128, 32, 64], F32))
                BIt = esh.enter_context(sb("BIt", [128, 32, 64], F32))
                T1 = esh.enter_context(sb("T1", [128, 32, 64], F32))
                T2 = esh.enter_context(sb("T2", [128, 32, 64], F32))
                P.dma('sp', LR[:], lam_re[:, i, :], [], ['LR'])
                P.dma('sp', LI[:], lam_im[:, i, :], [], ['LI'])
                P.dma('sp', LDT[:], log_dt[:, i, :], [], ['LDT'])
                P.dma('sp', BRt[:], b_re[:, i, :, :], [], ['BRt'])
                P.dma('sp', BIt[:], b_im[:, i, :, :], [], ['BIt'])
                P.dma('sp', CRt[:], c_re[:, i, :, :], [], ['CRt'])
                P.dma('sp', CIt[:], c_im[:, i, :, :], [], ['CIt'])
                P.dma('sp', ID2[:], ident2[:, :], [], ['ID2'])
                P.dma('sp', DV[:], ssm_d[:, :, :], [], ['DV'])
                P.op('dve', [], ['NPI'], lambda e: e.memset(NPI[:], -3.141592653589793))

                def tt_(out, a_, b_, op, rk, wk, eng='dve'):
                    P.op(eng, rk, wk, lambda e: e.tensor_tensor(out=out, in0=a_, in1=b_, op=op))

                P.op('act', ['LDT'], ['DTt'], lambda e: e.activation(out=DTt[:], in_=LDT[:], func=AF.Exp))
                tt_(LRD[:], LR[:], DTt[:], ALU.mult, ['LR', 'DTt'], ['LRD'])
                tt_(LID[:], LI[:], DTt[:], ALU.mult, ['LI', 'DTt'], ['LID'])
                P.op('act', ['LRD'], ['MAG'], lambda e: e.activation(out=MAG[:], in_=LRD[:], func=AF.Exp))

                def sincos(dst, dk, phase):
                    P.op('dve', ['LID'], ['U1'], lambda e: e.tensor_scalar(
                        out=U1[:], in0=LID[:], scalar1=1.0 / TWO_PI, scalar2=phase, op0=ALU.mult, op1=ALU.add))
                    P.op('dve', ['U1'], ['KI'], lambda e: e.tensor_copy(out=KI[:], in_=U1[:]))
                    P.op('dve', ['KI'], ['KF'], lambda e: e.tensor_copy(out=KF[:], in_=KI[:]))
                    tt_(FR[:], U1[:], KF[:], ALU.subtract, ['U1', 'KF'], ['FR'])
                    P.op('dve', ['FR'], ['NEGm'], lambda e: e.tensor_scalar(
                        out=NEGm[:], in0=FR[:], scalar1=0.0, scalar2=None, op0=ALU.is_lt))
                    tt_(FR[:], FR[:], NEGm[:], ALU.add, ['FR', 'NEGm'], ['FR'])
                    P.op('act', ['FR', 'NPI'], [dk], lambda e: e.activation(
                        out=dst[:], in_=FR[:], func=AF.Sin, scale=TWO_PI, bias=NPI[:, 0:1]))
                sincos(SINV, 'SINV', 0.5)
                sincos(COSV, 'COSV', 0.75)
                tt_(ABR[:], MAG[:], COSV[:], ALU.mult, ['MAG', 'COSV'], ['ABR'])
                tt_(ABI[:], MAG[:], SINV[:], ALU.mult, ['MAG', 'SINV'], ['ABI'])
                tt_(A1[:], LR[:], LR[:], ALU.mult, ['LR'], ['A1'])
                tt_(A2[:], LI[:], LI[:], ALU.mult, ['LI'], ['A2'])
                tt_(DEN[:], A1[:], A2[:], ALU.add, ['A1', 'A2'], ['DEN'])
                P.op('dve', ['DEN'], ['RDN'], lambda e: e.reciprocal(out=RDN[:], in_=DEN[:]))
                P.op('dve', ['ABR'], ['NRE'], lambda e: e.tensor_scalar(
                    out=NRE[:], in0=ABR[:], scalar1=-1.0, scalar2=None, op0=ALU.add))
                tt_(A1[:], NRE[:], LR[:], ALU.mult, ['NRE', 'LR'], ['A1'])
                tt_(A2[:], ABI[:], LI[:], ALU.mult, ['ABI', 'LI'], ['A2'])
                tt_(A1[:], A1[:], A2[:], ALU.add, ['A1', 'A2'], ['A1'])
                tt_(FRE[:], A1[:], RDN[:], ALU.mult, ['A1', 'RDN'], ['FRE'])
                tt_(A1[:], ABI[:], LR[:], ALU.mult, ['ABI', 'LR'], ['A1'])
                tt_(A2[:], NRE[:], LI[:], ALU.mult, ['NRE', 'LI'], ['A2'])
                tt_(A1[:], A1[:], A2[:], ALU.subtract, ['A1', 'A2'], ['A1'])
                tt_(FIM[:], A1[:], RDN[:], ALU.mult, ['A1', 'RDN'], ['FIM'])
                frb = FRE[:].unsqueeze(2).to_broadcast([128, 32, 64])
                fib = FIM[:].unsqueeze(2).to_broadcast([128, 32, 64])
                tt_(T1[:], BRt[:], frb, ALU.mult, ['BRt', 'FRE'], ['T1'])
                tt_(T2[:], BIt[:], fib, ALU.mult, ['BIt', 'FIM'], ['T2'], eng='pool')
                tt_(BBR[:], T1[:], T2[:], ALU.subtract, ['T1', 'T2'], ['BBR'])
                tt_(T1[:], BIt[:], frb, ALU.mult, ['BIt', 'FRE'], ['T1'])
                tt_(T2[:], BRt[:], fib, ALU.mult, ['BRt', 'FIM'], ['T2'], eng='pool')
                tt_(BBI[:], T1[:], T2[:], ALU.add, ['T1', 'T2'], ['BBI'])
                P.op('dve', [], ['PWR'], lambda e: e.memset(PWR[:, 0, :], 1.0))
                P.op('dve', [], ['PWI'], lambda e: e.memset(PWI[:, 0, :], 0.0))
                for k in range(8):
                    tt_(A1[:], PWR[:, k, :], ABR[:], ALU.mult, ['PWR', 'ABR'], ['A1'])
                    tt_(A2[:], PWI[:, k, :], ABI[:], ALU.mult, ['PWI', 'ABI'], ['A2'])
                    tt_(PWR[:, k + 1, :], A1[:], A2[:], ALU.subtract, ['A1', 'A2'], ['PWR'])
                    tt_(A1[:], PWR[:, k, :], ABI[:], ALU.mult, ['PWR', 'ABI'], ['A1'])
                    tt_(A2[:], PWI[:, k, :], ABR[:], ALU.mult, ['PWI', 'ABR'], ['A2'])
                    tt_(PWI[:, k + 1, :], A1[:], A2[:], ALU.add, ['A1', 'A2'], ['PWI'])
                P.op('dve', ['PWR'], ['AKS'], lambda e: e.tensor_copy(out=AKS[:, i, 0, 0, :], in_=PWR[:, 8, :]))
                P.op('dve', ['PWI'], ['AKS'], lambda e: e.tensor_copy(out=AKS[:, i, 1, 0, :], in_=PWI[:, 8, :]))
                P.op('dve', ['PWI'], ['AKS'], lambda e: e.tensor_scalar(
                    out=AKS[:, i, 2, 0, :], in0=PWI[:, 8, :], scalar1=-1.0, scalar2=None, op0=ALU.mult))
                for k in range(7):
                    tt_(A1[:], AKS[:, i, 0, k, :], AKS[:, i, 0, k, :], ALU.mult, ['AKS'], ['A1'])
                    tt_(A2[:], AKS[:, i, 1, k, :], AKS[:, i, 1, k, :], ALU.mult, ['AKS'], ['A2'])
                    tt_(U1[:], AKS[:, i, 0, k, :], AKS[:, i, 1, k, :], ALU.mult, ['AKS'], ['U1'])
                    tt_(AKS[:, i, 0, k + 1, :], A1[:], A2[:], ALU.subtract, ['A1', 'A2'], ['AKS'])
                    P.op('dve', ['U1'], ['AKS'], lambda e, k=k: e.tensor_scalar(
                        out=AKS[:, i, 1, k + 1, :], in0=U1[:], scalar1=2.0, scalar2=None, op0=ALU.mult))
                    P.op('dve', ['U1'], ['AKS'], lambda e, k=k: e.tensor_scalar(
                        out=AKS[:, i, 2, k + 1, :], in0=U1[:], scalar1=-2.0, scalar2=None, op0=ALU.mult))
                P.barrier()
                esh.close()
                TA2 = [es2.enter_context(sb("TA%d" % b_, [128, 9, 4, 64], F32)) for b_ in range(2)]
                TB2 = [es2.enter_context(sb("TB%d" % b_, [128, 9, 4, 64], F32)) for b_ in range(2)]
                SBb2 = [es2.enter_context(sb("SBb%d" % b_, [128, 2, 8, 4, 64], BF16)) for b_ in range(2)]
                CAb2 = [es2.enter_context(sb("CAb%d" % b_, [128, 2, 9, 4, 64], BF16)) for b_ in range(2)]
                OUTB = [es2.enter_context(sb("OUTB%d" % b, [128, 8704], BF16)) for b in range(2)]
                yield
                def stage_a(cc):
                    j0 = cc * 4
                    TA, TB, SBb, CAb = TA2[cc % 2], TB2[cc % 2], SBb2[cc % 2], CAb2[cc % 2]
                    sfx = '_%d' % (cc % 2)
                    ob = OUTB[cc % 2]
                    ok = 'OUTB%d' % (cc % 2)
                    pwr8 = PWR[:, 0:8, j0:j0 + 4].unsqueeze(3).to_broadcast([128, 8, 4, 64])
                    pwi8 = PWI[:, 0:8, j0:j0 + 4].unsqueeze(3).to_broadcast([128, 8, 4, 64])
                    pwr9 = PWR[:, 0:9, j0:j0 + 4].unsqueeze(3).to_broadcast([128, 9, 4, 64])
                    pwi9 = PWI[:, 0:9, j0:j0 + 4].unsqueeze(3).to_broadcast([128, 9, 4, 64])
                    bbr = BBR[:, j0:j0 + 4, :].unsqueeze(1).to_broadcast([128, 8, 4, 64])
                    bbi = BBI[:, j0:j0 + 4, :].unsqueeze(1).to_broadcast([128, 8, 4, 64])
                    cr9 = CRt[:, j0:j0 + 4, :].unsqueeze(1).to_broadcast([128, 9, 4, 64])
                    ci9 = CIt[:, j0:j0 + 4, :].unsqueeze(1).to_broadcast([128, 9, 4, 64])
                    tt_(TA[:, 0:8], bbr, pwr8, ALU.mult, ['BBR', 'PWR'], ['TA' + sfx])
                    tt_(TB[:, 0:8], bbi, pwi8, ALU.mult, ['BBI', 'PWI'], ['TB' + sfx], eng='pool')
                    tt_(SBb[:, 0], TA[:, 0:8], TB[:, 0:8], ALU.subtract, ['TA' + sfx, 'TB' + sfx], ['SBb0' + sfx])
                    tt_(TA[:, 0:8], bbi, pwr8, ALU.mult, ['BBI', 'PWR'], ['TA' + sfx])
                    tt_(TB[:, 0:8], bbr, pwi8, ALU.mult, ['BBR', 'PWI'], ['TB' + sfx])
                    tt_(SBb[:, 1], TA[:, 0:8], TB[:, 0:8], ALU.add, ['TA' + sfx, 'TB' + sfx], ['SBb1' + sfx])
                    tt_(TA[:], cr9, pwr9, ALU.mult, ['CRt', 'PWR'], ['TA' + sfx])
                    tt_(TB[:], ci9, pwi9, ALU.mult, ['CIt', 'PWI'], ['TB' + sfx])
                    tt_(CAb[:, 0], TA[:], TB[:], ALU.subtract, ['TA' + sfx, 'TB' + sfx], ['CAb0' + sfx])
                    tt_(TA[:], cr9, pwi9, ALU.mult, ['CRt', 'PWI'], ['TA' + sfx])
                    tt_(TB[:], ci9, pwr9, ALU.mult, ['CIt', 'PWR'], ['TB' + sfx], eng='pool')
                    P.op('dve', ['TA' + sfx, 'TB' + sfx], ['CAb1' + sfx], lambda e: e.scalar_tensor_tensor(
                        out=CAb[:, 1], in0=TA[:], scalar=-1.0, in1=TB[:], op0=ALU.mult, op1=ALU.subtract))
                def stage_b(cc):
                    j0 = cc * 4
                    TA, TB, SBb, CAb = TA2[cc % 2], TB2[cc % 2], SBb2[cc % 2], CAb2[cc % 2]
                    sfx = '_%d' % (cc % 2)
                    ob = OUTB[cc % 2]
                    ok = 'OUTB%d' % (cc % 2)
                    for tau in range(8):
                        pi = 2 + (tau % 2)
                        for q in range(4):
                            h2, w = q // 2, q % 2
                            hs = slice(64 * h2, 64 * h2 + 64)
                            P.op('pe', ['SBb0' + sfx, 'IDB'], ['PS%d_%d' % (pi, h2)],
                                 mm(PS[pi][hs, w * 256:w * 256 + 128], SBb[:, 0, tau, q, :], IDB[:], True, True))
                            P.op('pe', ['SBb1' + sfx, 'IDB'], ['PS%d_%d' % (pi, h2)],
                                 mm(PS[pi][hs, w * 256 + 128:w * 256 + 256], SBb[:, 1, tau, q, :], IDB[:], True, True))
                        P.op('act', ['PS%d_0' % pi, 'PS%d_1' % pi], [ok],
                             lambda e, ob=ob, tau=tau, pi=pi: e.copy(out=ob[:, tau * 512:(tau + 1) * 512], in_=PS[pi][:, 0:512]))
                    for tau in range(8):
                        for h2 in range(2):
                            hs = slice(64 * h2, 64 * h2 + 64)
                            for w in range(2):
                                q = 2 * h2 + w
                                P.op('pe', ['SBb0' + sfx, 'CAb0' + sfx], ['PS4_%d' % h2],
                                     mm(PS4[hs, tau * 64:(tau + 1) * 64], SBb[:, 0, 0, q, :], CAb[:, 0, tau, q, :], w == 0, False))
                                P.op('pe', ['SBb1' + sfx, 'CAb1' + sfx], ['PS4_%d' % h2],
                                     mm(PS4[hs, tau * 64:(tau + 1) * 64], SBb[:, 1, 0, q, :], CAb[:, 1, tau, q, :], False, w == 1))
                    P.op('dve', ['PS4_0', 'PS4_1', 'ID2', 'DV'], [ok], lambda e, ob=ob, cc=cc: e.scalar_tensor_tensor(
                        out=ob[:, 4096:4160], in0=ID2[:], scalar=DV[:, i, cc:cc + 1], in1=PS4[:, 0:64],
                        op0=ALU.mult, op1=ALU.add))
                    P.op('act', ['PS4_0', 'PS4_1'], [ok], lambda e, ob=ob: e.copy(out=ob[:, 4160:4608], in_=PS4[:, 64:512]))
                    for r in range(2):
                        P.op('act', ['CAb%d' % r + sfx], [ok], lambda e, ob=ob, r=r: e.copy(
                            out=ob[:, 4608 + r * 2048:4608 + (r + 1) * 2048].rearrange("p (t q c) -> p t q c", t=8, q=4),
                            in_=CAb[:, r, 1:9]))
                    P.dma('act', ssm_scr[i, cc, :, :], ob[:], [ok], ['SCR%d_%d' % (i, cc)])
                stage_a(0)
                for cc in range(8):
                    if cc + 1 < 8:
                        stage_a(cc + 1)
                    stage_b(cc)
                    yield
                P.barrier()

        ada_phase([emit_ssm_prep(i_) for i_ in sorted(set(l_ // 2 for l_ in layers if l_ % 2 == 1))])

        def ada_vec(l, idx, s, kc):
            return ADA[:, l, idx * 8 + kc, s:s + 1]

        def emit_dispatch(l, es, GTS, OHS, OHF):
            def t(nm, w, dt=F32):
                return es.enter_context(sb(nm, [128, w], dt))
            CNT, X1, U2, KF2, FR2, NG2, FL, PC, PEND, PST, ONE32 = [t(n, 32) for n in
                ("CNT", "X1", "U2", "KF2", "FR2", "NG2", "FL", "PC", "PEND", "PST", "ONE32")]
            KI2 = t("KI2", 32, I32)
            VAL = t("VAL", 32)
            VAL1 = t("VAL1", 32)
            JNK = t("JNK", 32)
            M8d = t("M8d", 8)
            DP1 = t("DP1", 32)
            GSEL = t("GSEL", 32)
            DST, DQ, DU, DKF, DFR, DNG, D2 = [t(n, 32) for n in ("DST", "DQ", "DU", "DKF", "DFR", "DNG", "D2")]
            DKI = t("DKI", 32, I32)
            D2I = t("D2I", 32, I32)
            PAYI = es.enter_context(sb("PAYI", [128, 16, 2, 8], I32))
            TOKI = t("TOKI", 16, I32)
            RIN = t("RIN", 512, I32)
            B128 = t("B128", 64)
            PIDX = t("PIDX", 1)
            CMP = es.enter_context(sb("CMP", [128, 64, 32], F32))
            BE, SK, EM, IW = [t(n, 64) for n in ("BE", "SK", "EM", "IW")]
            P.dma('sp', TOKI[:], toki[:, :], [], ['TOKI'])
            P.dma('sp', RIN[:], rinit[:, :], [], ['RIN'])
            P.dma('sp', B128[:], b128[:, :], [], ['B128'])
            P.dma('sp', PIDX[:], pidx[:, :], [], ['PIDX'])
            P.op('dve', [], ['ONE32'], lambda e: e.memset(ONE32[:], 1.0))
            ohk = ['OHS%d' % n for n in range(16)]
            for n in range(16):
                P.op('pe', ['ONESB', 'OHS%d' % n], ['PS3'], mm(PS3[:, 0:32], ONESB[:], OHS[:, n, :], n == 0, n == 15))
            P.op('dve', ['PS3'], ['CNT'], lambda e: e.tensor_copy(out=CNT[:], in_=PS3[:, 0:32]))

            def floor128(dst, dk, srcap, sk, U, KI, KF, FRc, NG, pre):
                P.op('dve', [sk], [pre + 'U'], lambda e: e.tensor_scalar(out=U[:], in0=srcap, scalar1=1.0 / 128, scalar2=None, op0=ALU.mult))
                P.op('dve', [pre + 'U'], [pre + 'KI'], lambda e: e.tensor_copy(out=KI[:], in_=U[:]))
                P.op('dve', [pre + 'KI'], [pre + 'KF'], lambda e: e.tensor_copy(out=KF[:], in_=KI[:]))
                P.op('dve', [pre + 'U', pre + 'KF'], [pre + 'FR'], lambda e: e.tensor_tensor(out=FRc[:], in0=U[:], in1=KF[:], op=ALU.subtract))
                P.op('dve', [pre + 'FR'], [pre + 'NG'], lambda e: e.tensor_scalar(out=NG[:], in0=FRc[:], scalar1=0.0, scalar2=None, op0=ALU.is_lt))
                P.op('dve', [pre + 'KF', pre + 'NG'], [dk], lambda e: e.tensor_tensor(out=dst[:], in0=KF[:], in1=NG[:], op=ALU.subtract))
            P.op('dve', ['CNT'], ['X1'], lambda e: e.tensor_scalar(out=X1[:], in0=CNT[:], scalar1=127.0, scalar2=None, op0=ALU.add))
            floor128(FL, 'FL', X1[:], 'X1', U2, KI2, KF2, FR2, NG2, 'a')
            P.op('dve', ['FL'], ['PC'], lambda e: e.tensor_scalar(out=PC[:], in0=FL[:], scalar1=128.0, scalar2=None, op0=ALU.mult))
            P.op('dve', ['PC', 'ONE32'], ['PEND'], lambda e: e.tensor_tensor_scan(
                out=PEND[:], data0=ONE32[:], data1=PC[:], initial=0.0, op0=ALU.mult, op1=ALU.add))
            P.op('dve', ['PC', 'PEND'], ['PST'], lambda e: e.scalar_tensor_tensor(
                out=PST[:], in0=PC[:], scalar=-1.0, in1=PEND[:], op0=ALU.mult, op1=ALU.add))
            VA = es.enter_context(sb("VA", [128, 16, 32], F32))
            VB = es.enter_context(sb("VB", [128, 16, 32], F32))
            MH = es.enter_context(sb("MH", [128, 16, 32], F32))
            ML = es.enter_context(sb("ML", [128, 16, 32], F32))
            TQ = es.enter_context(sb("TQ", [128, 16, 32], F32))
            for n in range(16):
                first = True
                for n2 in range(n):
                    P.op('pe', ['ONESB', 'OHS%d' % n2], ['PS4'], mm(PS4[:, n * 32:(n + 1) * 32], ONESB[:], OHS[:, n2, :], first, False))
                    first = False
                P.op('pe', ['TRIB', 'OHS%d' % n], ['PS4'], mm(PS4[:, n * 32:(n + 1) * 32], TRIB[:], OHS[:, n, :], first, True))
            ofk = ['OHF%d' % n for n in range(16)]
            gtk = ['GTS%d' % n for n in range(16)]

            def b16(ap2):
                return ap2.unsqueeze(2).to_broadcast([128, 16, 32])
            DP3 = DP1[:].rearrange("p (n k) -> p n k", k=2)
            GS3 = GSEL[:].rearrange("p (n k) -> p n k", k=2)
            DHI, DLO, GHI, GLO = [t(nm, 16) for nm in ("DHI", "DLO", "GHI", "GLO")]
            P.op('dve', ['PS4', 'PST'], ['VA'], lambda e: e.tensor_tensor(
                out=VA[:], in0=PS4[:].rearrange("p (n c) -> p n c", n=16), in1=PST[:].unsqueeze(1).to_broadcast([128, 16, 32]), op=ALU.add))
            P.op('dve', ['VA'] + ofk, ['VA'], lambda e: e.scalar_tensor_tensor(
                out=VA[:], in0=VA[:], scalar=1.0, in1=OHF[:], op0=ALU.add, op1=ALU.mult))
            P.op('dve', ['VA'], ['DHI'], lambda e: e.tensor_reduce(out=DHI[:], in_=VA[:], axis=AX.X, op=ALU.max))
            P.op('dve', ['VA', 'DHI'], ['MH'], lambda e: e.tensor_tensor(out=MH[:], in0=VA[:], in1=b16(DHI[:]), op=ALU.is_equal))
            P.op('dve', ['MH', 'VA'], ['TQ'], lambda e: e.tensor_tensor(out=TQ[:], in0=MH[:], in1=VA[:], op=ALU.mult))
            P.op('dve', ['VA', 'TQ'], ['VB'], lambda e: e.tensor_tensor(out=VB[:], in0=VA[:], in1=TQ[:], op=ALU.subtract))
            P.op('dve', ['VB'], ['DLO'], lambda e: e.tensor_reduce(out=DLO[:], in_=VB[:], axis=AX.X, op=ALU.max))
            P.op('dve', ['VB', 'DLO'], ['ML'], lambda e: e.tensor_tensor(out=ML[:], in0=VB[:], in1=b16(DLO[:]), op=ALU.is_equal))
            P.op('dve', ['MH'] + gtk, ['TQ'], lambda e: e.tensor_tensor(out=TQ[:], in0=MH[:], in1=GTS[:], op=ALU.mult))
            P.op('dve', ['TQ'], ['GHI'], lambda e: e.tensor_reduce(out=GHI[:], in_=TQ[:], axis=AX.X, op=ALU.add))
            P.op('dve', ['ML', 'TQ'] + gtk, ['TQ'], lambda e: e.tensor_tensor(out=TQ[:], in0=ML[:], in1=GTS[:], op=ALU.mult))
            P.op('dve', ['TQ'], ['GLO'], lambda e: e.tensor_reduce(out=GLO[:], in_=TQ[:], axis=AX.X, op=ALU.add))
            P.op('dve', ['DHI'], ['DP1'], lambda e: e.tensor_copy(out=DP3[:, :, 0], in_=DHI[:]))
            P.op('dve', ['DLO', 'DP1'], ['DP1'], lambda e: e.tensor_copy(out=DP3[:, :, 1], in_=DLO[:]))
            P.op('dve', ['GHI'], ['GSEL'], lambda e: e.tensor_copy(out=GS3[:, :, 0], in_=GHI[:]))
            P.op('dve', ['GLO', 'GSEL'], ['GSEL'], lambda e: e.tensor_copy(out=GS3[:, :, 1], in_=GLO[:]))
            dpk = ['DP1']
            gsk = ['GSEL']
            P.op('dve', dpk, ['DST'], lambda e: e.tensor_scalar(out=DST[:], in0=DP1[:], scalar1=-1.0, scalar2=None, op0=ALU.add))
            P.op('dve', ['DST'], ['DSTI'], lambda e: e.tensor_copy(out=DSTI[:], in_=DST[:]))
            floor128(DQ, 'DQ', DST[:], 'DST', DU, DKI, DKF, DFR, DNG, 'b')
            P.op('dve', ['DQ', 'DST'], ['D2'], lambda e: e.scalar_tensor_tensor(
                out=D2[:], in0=DQ[:], scalar=-128.0, in1=DST[:], op0=ALU.mult, op1=ALU.add))
            P.op('dve', ['D2', 'DQ'], ['D2'], lambda e: e.scalar_tensor_tensor(
                out=D2[:], in0=D2[:], scalar=64.0, in1=DQ[:], op0=ALU.mult, op1=ALU.add))
            P.op('dve', ['D2'], ['D2I'], lambda e: e.tensor_copy(out=D2I[:], in_=D2[:]))
            P.op('dve', [], ['PAY0', 'PAY1'], lambda e: e.memset(PAYI[:], 0))
            P.op('dve', ['TOKI', 'PAY0'], ['PAY0'], lambda e: e.tensor_copy(
                out=PAYI[:, :, :, 0], in_=TOKI[:].unsqueeze(2).to_broadcast([128, 16, 2])))
            P.op('dve', gsk + ['PAY1'], ['PAY1'], lambda e: e.tensor_copy(
                out=PAYI[:].bitcast(F32)[:, :, :, 1], in_=GSEL[:].rearrange("p (n k) -> p n k", k=2)))
            P.dma('sp', ROWT.rearrange("(p x) c -> p (x c)", p=128), RIN[:], ['RIN'], ['ROWT'])
            if 'rowt' not in _regs:
                _regs['rowt'] = nc.gpsimd.alloc_register(name="rowtmax_reg")
                nc.gpsimd.reg_mov(_regs['rowt'], 8191)
            for n in range(16):
                for k in range(2):
                    P.idma('pool', lambda g, n=n, k=k: g.indirect_dma_start(
                        out=ROWT[:, :], out_offset=bass.IndirectOffsetOnAxis(ap=D2I[:, 2 * n + k:2 * n + k + 1], axis=0),
                        in_=PAYI[:, n, k, :], in_offset=None, bounds_check=_regs['rowt'], oob_is_err=False),
                        ['D2I', 'PAY0', 'PAY1', 'ROWT'], ['ROWTs%d_%d' % (n, k)])
            P.dma('sp', IDXG[:].rearrange("p b c -> p (b c)"), ROWT.rearrange("(p x) c -> p (x c)", p=128), ['ROWT'] + ['ROWTs%d_%d' % (n, k) for n in range(16) for k in range(2)], ['IDXG'])
            P.op('dve', ['PEND', 'B128'], ['CMP'], lambda e: e.tensor_tensor(
                out=CMP[:], in0=PEND[:].unsqueeze(1).to_broadcast([128, 64, 32]),
                in1=B128[:].unsqueeze(2).to_broadcast([128, 64, 32]), op=ALU.is_le))
            P.op('dve', ['CMP'], ['BE'], lambda e: e.tensor_reduce(out=BE[:], in_=CMP[:], axis=AX.X, op=ALU.add))
            P.op('dve', ['BE'], ['BE'], lambda e: e.tensor_scalar(out=BE[:], in0=BE[:], scalar1=31.0, scalar2=None, op0=ALU.min))
            P.op('dve', [], ['SK'], lambda e: e.memset(SK[:], 0.0))
            P.op('dve', ['BE', 'SK'], ['SK'], lambda e: e.tensor_tensor(out=SK[:, 1:64], in0=BE[:, 1:64], in1=BE[:, 0:63], op=ALU.is_equal))
            for rb in (MOE_R0, MOE_R0 + MOE_R1):
                P.op('dve', ['SK'], ['SK'], lambda e, rb=rb: e.memset(SK[:, rb:rb + 1], 0.0))
            P.op('dve', ['B128', 'PEND'], ['EM'], lambda e: e.tensor_scalar(out=EM[:], in0=B128[:], scalar1=PEND[:, 31:32], scalar2=None, op0=ALU.is_ge))
            P.op('dve', ['SK', 'EM'], ['SK'], lambda e: e.tensor_tensor(out=SK[:], in0=SK[:], in1=EM[:], op=ALU.max))
            P.op('dve', ['BE', 'PIDX'], ['IW'], lambda e: e.tensor_scalar(
                out=IW[:], in0=BE[:], scalar1=128.0, scalar2=PIDX[:, 0:1], op0=ALU.mult, op1=ALU.add))
            P.op('dve', ['IW', 'SK'], ['IW'], lambda e: e.scalar_tensor_tensor(
                out=IW[:], in0=SK[:], scalar=1.0e6, in1=IW[:], op0=ALU.mult, op1=ALU.add))
            P.op('dve', ['IW'], ['IDXW'], lambda e: e.tensor_scalar(
                out=IDXW[:], in0=IW[:], scalar1=float(4096 * l), scalar2=None, op0=ALU.add))

        def emit_norm(l, s, which, router=None):
            shift_idx = 0 if which == 0 else 3
            with ExitStack() as es:
                SQ2b = [es.enter_context(sb("SQ%d" % b_, [128, 8, 512], BF16)) for b_ in range(2)]
                SD2b = [es.enter_context(sb("SD%d" % b_, [128, 512], F32)) for b_ in range(2)]
                RS2b = [es.enter_context(sb("RS%d" % b_, [128, 512], F32)) for b_ in range(2)]
                TMP0 = es.enter_context(sb("TMP0", [128, 512], F32))
                TMP1 = es.enter_context(sb("TMP1", [128, 512], F32))
                TMP = [TMP0, TMP1]
                cnt = 0
                if which == 1:
                    HL2 = [es.enter_context(sb("HL%d" % b_, [128, 8, 512], BF16)) for b_ in range(2)]
                    WRF = es.enter_context(sb("WRF", [128, 8, 36], F32))
                    WRH = es.enter_context(sb("WRH", [128, 8, 36], BF16))
                    WRL = es.enter_context(sb("WRL", [128, 8, 36], BF16))
                    L36 = es.enter_context(sb("L36", [128, 36], F32))
                    RT = es.enter_context(sb("RT", [128, 16], F32))
                    OG = es.enter_context(sb("OG", [128, 4], F32))
                    PEN = es.enter_context(sb("PEN", [128, 4], F32))
                    GE = es.enter_context(sb("GE", [128, 4], F32))
                    LM = es.enter_context(sb("LM", [128, 32], F32))
                    M8 = es.enter_context(sb("M8", [128, 8], F32))
                    SELM = es.enter_context(sb("SELM", [128, 32], F32))
                    EX = es.enter_context(sb("EX", [128, 32], F32))
                    EXS = es.enter_context(sb("EXS", [128, 32], F32))
                    GATES = es.enter_context(sb("GATES", [128, 32], F32))
                    if SPARSE:
                        GTS = es.enter_context(sb("GTS", [128, 16, 32], F32))
                        OHS = es.enter_context(sb("OHS", [128, 16, 32], BF16))
                        OHF = es.enter_context(sb("OHF", [128, 16, 32], F32))
                        HROW = [es.enter_context(sb("HROW%d" % b, [128, 1024], BF16)) for b in range(2)]
                    P.dma('sp', WRF[:], wr[l, :, :, :], [], ['WRF'])
                    P.op('dve', ['WRF'], ['WRH'], lambda e: e.tensor_copy(out=WRH[:], in_=WRF[:]))
                    P.op('dve', ['WRF', 'WRH'], ['WRL'],
                         lambda e: e.tensor_tensor(out=WRL[:], in0=WRF[:], in1=WRH[:], op=ALU.subtract))

                    L4 = es.enter_context(sb("L4", [128, 4, 36], F32))
                    R4 = es.enter_context(sb("R4", [128, 8, 4], F32))
                    LGS = es.enter_context(sb("LGS", [128, 4, 4], F32))
                    GE4 = es.enter_context(sb("GE4", [128, 4, 4], F32))
                    OG4 = es.enter_context(sb("OG4", [128, 4, 4], F32))
                    PEN4 = es.enter_context(sb("PEN4", [128, 4, 4], F32))
                    LM4 = es.enter_context(sb("LM4", [128, 4, 32], F32))
                    MK1 = es.enter_context(sb("MK1", [128, 4, 32], F32))
                    LM2 = es.enter_context(sb("LM2", [128, 4, 32], F32))
                    LMS = es.enter_context(sb("LMS", [128, 4, 32], F32))
                    EX4 = es.enter_context(sb("EX4", [128, 4, 32], F32))
                    EXS4 = es.enter_context(sb("EXS4", [128, 4, 32], F32))

                    def emit_router(l, blk):
                        n4 = slice(blk * 4, blk * 4 + 4)
                        for tt in range(4):
                            n = blk * 4 + tt
                            ts_ = slice(n * 128, (n + 1) * 128)
                            tl = slice(tt * 128, (tt + 1) * 128)
                            pc = slice(tt * 64, tt * 64 + 36)
                            first = True
                            for kc in range(8):
                                for (A, ak, asl, Wt, wk) in ((HT, 'HT%d_%d' % (kc, blk), ts_, WRH, 'WRH'),
                                                             (HL2[blk % 2], 'HL%d_%d' % (blk % 2, kc), tl, WRH, 'WRH'),
                                                             (HT, 'HT%d_%d' % (kc, blk), ts_, WRL, 'WRL')):
                                    P.op('pe', [ak, wk], ['PS3'],
                                         mm(PS3[:, pc], A[:, kc, asl], Wt[:, kc, :], first, False))
                                    first = False
                            P.op('pe', ['ONEF', 'BR'], ['PS3'],
                                 mm(PS3[:, pc], ONEF[0:1, :], BR[0:1, l, :], False, True))
                            PSB = PS7[:].bitcast(BF16)
                            for kc in range(8):
                                P.op('pe', ['HT%d_%d' % (kc, blk), 'IDB'], ['PS7'],
                                     lambda pe, kc=kc, ts_=ts_: pe.transpose(PSB[:, kc * 128:(kc + 1) * 128], HT[:, kc, ts_], IDB[:]))
                            hb = n % 2
                            P.op('act', ['PS7'], ['HROW%d' % hb], lambda e, hb=hb: e.copy(out=HROW[hb][:], in_=PSB))
                            P.dma('sp', HD[n * 128:(n + 1) * 128, :], HROW[hb][:], ['HROW%d' % hb], ['HD'])

                        def bc(ap2, w):
                            return ap2.unsqueeze(2).to_broadcast([128, 4, w])
                        GM, GS, M1, M2, SS, DEN, RDEN = [R4[:, j, :] for j in range(7)]
                        okk = ['OHF%d' % n for n in range(blk * 4, blk * 4 + 4)]
                        gkk = ['GTS%d' % n for n in range(blk * 4, blk * 4 + 4)]
                        ohk = ['OHS%d' % n for n in range(blk * 4, blk * 4 + 4)]
                        P.op('dve', ['PS3'], ['L4'], lambda e: e.tensor_copy(
                            out=L4[:], in_=PS3[:, 0:256].rearrange("p (t c) -> p t c", t=4)[:, :, 0:36]))
                        P.op('dve', ['L4'], ['GM'], lambda e: e.tensor_reduce(out=GM, in_=L4[:, :, 0:4], axis=AX.X, op=ALU.max))
                        P.op('dve', ['L4', 'GM'], ['LGS'], lambda e: e.tensor_tensor(out=LGS[:], in0=L4[:, :, 0:4], in1=bc(GM, 4), op=ALU.subtract))
                        P.op('act', ['LGS'], ['GE4'], lambda e: e.activation(out=GE4[:], in_=LGS[:], func=AF.Exp))
                        P.op('dve', ['GE4'], ['GS'], lambda e: e.tensor_reduce(out=GS, in_=GE4[:], axis=AX.X, op=ALU.add))
                        P.op('dve', ['L4', 'GM'], ['OG4'], lambda e: e.tensor_tensor(out=OG4[:], in0=L4[:, :, 0:4], in1=bc(GM, 4), op=ALU.is_equal))
                        P.op('dve', ['OG4'], ['PEN4'], lambda e: e.tensor_scalar(
                            out=PEN4[:], in0=OG4[:], scalar1=-1.0, scalar2=1e30, op0=ALU.add, op1=ALU.mult))
                        P.op('dve', ['L4', 'PEN4'], ['LM4'], lambda e: e.tensor_tensor(
                            out=LM4[:].rearrange("p t (g e) -> p t g e", g=4),
                            in0=L4[:, :, 4:36].rearrange("p t (g e) -> p t g e", g=4),
                            in1=PEN4[:].unsqueeze(3).to_broadcast([128, 4, 4, 8]), op=ALU.add))
                        P.op('dve', ['LM4'], ['M1'], lambda e: e.tensor_reduce(out=M1, in_=LM4[:], axis=AX.X, op=ALU.max))
                        P.op('dve', ['LM4', 'M1'], ['MK1'], lambda e: e.tensor_tensor(out=MK1[:], in0=LM4[:], in1=bc(M1, 32), op=ALU.is_equal))
                        P.op('dve', ['MK1', 'LM4'], ['LM2'], lambda e: e.scalar_tensor_tensor(
                            out=LM2[:], in0=MK1[:], scalar=-1e30, in1=LM4[:], op0=ALU.mult, op1=ALU.add))
                        P.op('dve', ['LM2'], ['M2'], lambda e: e.tensor_reduce(out=M2, in_=LM2[:], axis=AX.X, op=ALU.max))
                        P.op('dve', ['LM4', 'M2'], okk, lambda e: e.tensor_tensor(out=OHF[:, n4, :], in0=LM4[:], in1=bc(M2, 32), op=ALU.is_ge))
                        P.op('act', okk, ohk, lambda e: e.copy(out=OHS[:, n4, :], in_=OHF[:, n4, :]))
                        P.op('dve', ['LM4', 'M1'], ['LMS'], lambda e: e.tensor_tensor(out=LMS[:], in0=LM4[:], in1=bc(M1, 32), op=ALU.subtract))
                        P.op('act', ['LMS'], ['EX4'], lambda e: e.activation(out=EX4[:], in_=LMS[:], func=AF.Exp))
                        P.op('dve', ['EX4'] + okk, ['EXS4'], lambda e: e.tensor_tensor(out=EXS4[:], in0=EX4[:], in1=OHF[:, n4, :], op=ALU.mult))
                        P.op('dve', ['EXS4'], ['SS'], lambda e: e.tensor_reduce(out=SS, in_=EXS4[:], axis=AX.X, op=ALU.add))
                        P.op('dve', ['SS', 'GS'], ['DEN'], lambda e: e.tensor_tensor(out=DEN, in0=SS, in1=GS, op=ALU.mult))
                        P.op('dve', ['DEN'], ['RDEN'], lambda e: e.reciprocal(out=RDEN, in_=DEN))
                        P.op('dve', ['EXS4', 'RDEN'], gkk, lambda e: e.tensor_tensor(out=GTS[:, n4, :], in0=EXS4[:], in1=bc(RDEN, 32), op=ALU.mult))
                for blk in range(4):
                    cs = slice(blk * 512, (blk + 1) * 512)
                    SQ, SD, RS = SQ2b[blk % 2], SD2b[blk % 2], RS2b[blk % 2]
                    sqk, sdk, rsk = 'SQ%d_' % (blk % 2), 'SD%d' % (blk % 2), 'RS%d' % (blk % 2)
                    psn = PS[1 + (blk % 2)]
                    psk = 'PS%d' % (1 + (blk % 2))
                    for kc in range(8):
                        P.op('act', ['XT', 'XTL%d' % kc], [sqk + str(kc)],
                             lambda e, kc=kc, cs=cs, SQ=SQ: e.activation(out=SQ[:, kc, :], in_=XT[:, kc, cs],
                                                                         func=AF.Square))
                    for kc in range(8):
                        P.op('pe', [sqk + str(kc), 'ONESB'], [psk],
                             mm(psn[:], ONESB[:], SQ[:, kc, :], kc == 0, kc == 7))
                    P.op('act', [psk], [sdk],
                         lambda e, SD=SD, psn=psn: e.activation(out=SD[:], in_=psn[:], func=AF.Sqrt,
                                                                scale=1.0 / D, bias=EPSB[:, 0:1]))
                    P.op('dve', [sdk], [rsk], lambda e, SD=SD, RS=RS: e.reciprocal(out=RS[:], in_=SD[:]))
                    for kc in range(8):
                        tk = 'TMP%d' % (cnt % 2)
                        TT = TMP[cnt % 2]
                        cnt += 1
                        P.op('dve', ['XT', 'XTL%d' % kc, rsk, 'SCL'], [tk],
                             lambda e, kc=kc, cs=cs, TT=TT, RS=RS: e.scalar_tensor_tensor(
                                 out=TT[:], in0=XT[:, kc, cs], scalar=SCL[:, l, which, s, kc:kc + 1],
                                 in1=RS[:], op0=ALU.mult, op1=ALU.mult))
                        P.op('act', [tk, 'ADA'], ['HT%d_%d' % (kc, blk)],
                             lambda e, kc=kc, cs=cs, TT=TT: e.activation(
                                 out=HT[:, kc, cs], in_=TT[:], func=AF.Identity,
                                 bias=ada_vec(l, shift_idx, s, kc), scale=1.0))
                        if which == 1:
                            P.op('dve', [tk, 'ADA', 'HT%d_%d' % (kc, blk)], ['HL%d_%d' % (blk % 2, kc)],
                                 lambda e, kc=kc, cs=cs, TT=TT: e.scalar_tensor_tensor(
                                     out=HL2[blk % 2][:, kc, :], in0=TT[:], scalar=ada_vec(l, shift_idx, s, kc),
                                     in1=HT[:, kc, cs], op0=ALU.add, op1=ALU.subtract))
                    if which == 1 and blk >= 1:
                        emit_router(l, blk - 1)
                if which == 1:
                    emit_router(l, 3)
                if which == 1 and SPARSE:
                    emit_dispatch(l, es, GTS, OHS, OHF)
                P.barrier()

        with ExitStack() as es:
            EPSB = es.enter_context(sb("EPSB", [128, 4], F32))
            XT = es.enter_context(sb("XT", [128, 8, T], F32))
            HT = es.enter_context(sb("HT", [128, 8, T], BF16))
            P.op('dve', [], ['EPSB'], lambda e: e.memset(EPSB[:, 0:1], EPS))
            P.op('dve', [], ['EPSB'], lambda e: e.memset(EPSB[:, 1:2], 64.0 * EPS))
            P.op('dve', [], ['EPSB'], lambda e: e.memset(EPSB[:, 2:3], 0.0))
            P.barrier()

            def emit_attn(i, l, s):
                with ExitStack() as es:
                    QT = es.enter_context(sb("QT", [128, 8, 512], BF16))
                    KT = es.enter_context(sb("KT", [128, 2, T], BF16))
                    V = es.enter_context(sb("V", [128, 16, 256], BF16))
                    OT = es.enter_context(sb("OT", [128, 2, 4, 512], BF16))
                    E0 = es.enter_context(sb("E0", [128, 512], BF16))
                    E1 = es.enter_context(sb("E1", [128, 512], BF16))
                    E2 = es.enter_context(sb("E2", [128, 512], BF16))
                    E3 = es.enter_context(sb("E3", [128, 512], BF16))
                    QG = es.enter_context(sb("QG", [128, 2], F32))
                    KG = es.enter_context(sb("KG", [128, 2], F32))
                    XS = es.enter_context(sb("XS", [128, 2, 512], F32))
                    MB = es.enter_context(sb("MB", [128, 2, 512], BF16))
                    SQ2 = es.enter_context(sb("SQ2", [128, 512], BF16))
                    SD2 = es.enter_context(sb("SD2", [128, 512], F32))
                    RS2 = es.enter_context(sb("RS2", [128, 512], F32))
                    DS = es.enter_context(sb("DS", [128, 512], F32))
                    RD = es.enter_context(sb("RD", [128, 512], F32))
                    EB = [E0, E1, E2, E3]
                    P.dma('sp', QG[:], qgain[:, :], [], ['QG'])
                    P.dma('sp', KG[:], kgain[:, :], [], ['KG'])
                    P.dma('sp', XS[:], sinks[i, :, :, :], [], ['XSraw'])
                    P.dma('sp', MB[:], maskb[:, :, :], [], ['MB'])
                    P.op('act', ['XSraw'], ['XS'], lambda e: e.activation(out=XS[:], in_=XS[:], func=AF.Exp))
                    pq = 0
                    ecnt = 0
                    for blk in range(4):
                        cs = slice(blk * 512, (blk + 1) * 512)
                        hkeys = ['HT%d_%d' % (kc, blk) for kc in range(8)]
                        for c in range(10):
                            pk = 'PS%d' % (pq % 2)
                            pst = PS[pq % 2]
                            pq += 1
                            for kc in range(8):
                                P.op('pe', ['WQ%d' % kc] + hkeys, [pk],
                                     mm(pst[:], WQ[:, kc, c * 128:(c + 1) * 128], HT[:, kc, cs], kc == 0, kc == 7))
                            P.op('act', [pk], ['SQ2'],
                                 lambda e, pst=pst: e.activation(out=SQ2[:], in_=pst[:], func=AF.Square))
                            P.op('pe', ['SQ2', 'ONEBLK'], ['PS2'], mm(PS2[:], ONEBLK[:], SQ2[:], True, True))
                            if c < 8:
                                P.op('act', ['PS2'], ['SD2'],
                                     lambda e: e.activation(out=SD2[:], in_=PS2[:], func=AF.Sqrt,
                                                            scale=1.0, bias=EPSB[:, 1:2]))
                            else:
                                P.op('act', ['PS2'], ['SD2'],
                                     lambda e: e.activation(out=SD2[:], in_=PS2[:], func=AF.Sqrt,
                                                            scale=1.0 / 64, bias=EPSB[:, 0:1]))
                            P.op('dve', ['SD2'], ['RS2'], lambda e: e.reciprocal(out=RS2[:], in_=SD2[:]))
                            if c < 8:
                                P.op('dve', [pk, 'RS2', 'QG'], ['QT%d' % c],
                                     lambda e, pst=pst, c=c: e.scalar_tensor_tensor(
                                         out=QT[:, c, :], in0=pst[:], scalar=QG[:, i:i + 1], in1=RS2[:],
                                         op0=ALU.mult, op1=ALU.mult))
                            else:
                                P.op('dve', [pk, 'RS2', 'KG'], ['KT%d_%d' % (c - 8, blk)],
                                     lambda e, pst=pst, c=c, cs=cs: e.scalar_tensor_tensor(
                                         out=KT[:, c - 8, cs], in0=pst[:], scalar=KG[:, i:i + 1], in1=RS2[:],
                                         op0=ALU.mult, op1=ALU.mult))
                        for tt in range(4):
                            n = blk * 4 + tt
                            pk = 'PS%d' % (pq % 2)
                            pst = PS[pq % 2]
                            pq += 1
                            for kc in range(8):
                                P.op('pe', ['WQ%d' % kc] + hkeys, [pk],
                                     mm(pst[:, 0:256], HT[:, kc, n * 128:(n + 1) * 128], WQ[:, kc, 1280:1536],
                                        kc == 0, kc == 7))
                            P.op('act', [pk], ['V%d' % n],
                                 lambda e, pst=pst, n=n: e.copy(out=V[:, n, :], in_=pst[:, 0:256]))
                        items = []
                        for tt in range(4):
                            n = blk * 4 + tt
                            for i2 in range(2):
                                for hh in range(2):
                                    cks = [1] if n == 0 else [0, 1]
                                    for ci, ck in enumerate(cks):
                                        items.append((tt, n, i2, hh, ck, ci == 0, ci == len(cks) - 1))

                        def emit_scores(item, ebuf, psb):
                            tt, n, i2, hh, ck, first, last = item
                            nk = n - 1 + ck
                            hs = slice(64 * hh, 64 * hh + 64)
                            pst = PS[3 + psb]
                            pk = 'PS%d' % (3 + psb)
                            rhs = QT[hs, 4 * i2:4 * i2 + 4, tt * 128:(tt + 1) * 128]
                            P.op('pe', ['KT%d_%d' % (i2, nk // 4)] + ['QT%d' % (4 * i2 + g) for g in range(4)], [pk],
                                 mm(pst[:].rearrange("p (g q) -> p g q", g=4), KT[hs, i2, nk * 128:(nk + 1) * 128],
                                    rhs, True, False))
                            P.op('pe', ['IDB', 'MB'], [pk], mm(pst[:], IDB[:], MB[:, ck, :], False, True))
                            P.op('act', [pk], ['E%d' % ebuf],
                                 lambda e, pst=pst, ebuf=ebuf: e.activation(out=EB[ebuf][:], in_=pst[:], func=AF.Exp))

                        def emit_pv(item, ebuf):
                            tt, n, i2, hh, ck, first, last = item
                            nk = n - 1 + ck
                            h = 2 * i2 + hh
                            hs = slice(64 * hh, 64 * hh + 64)
                            P.op('pe', ['V%d' % nk, 'E%d' % ebuf], ['PS5_%d' % hh],
                                 mm(PS5[hs, :], V[:, nk, h * 64:(h + 1) * 64], EB[ebuf][:], first, last))
                            P.op('pe', ['ONESB', 'E%d' % ebuf], ['PS6_%d' % hh],
                                 mm(PS6[hs, :], ONESB[:, 0:64], EB[ebuf][:], first, last))
                            if last and hh == 1:
                                P.op('dve', ['PS6_0', 'PS6_1', 'XS'], ['DS'],
                                     lambda e, i2=i2: e.tensor_tensor(out=DS[:], in0=PS6[:], in1=XS[:, i2, :], op=ALU.add))
                                P.op('dve', ['DS'], ['RD'], lambda e: e.reciprocal(out=RD[:], in_=DS[:]))
                                P.op('dve', ['PS5_0', 'PS5_1', 'RD'], ['OT%d' % i2],
                                     lambda e, i2=i2, tt=tt: e.tensor_tensor(
                                         out=OT[:, i2, :, tt * 128:(tt + 1) * 128],
                                         in0=PS5[:].rearrange("p (g q) -> p g q", g=4),
                                         in1=RD[:].rearrange("p (g q) -> p g q", g=4), op=ALU.mult))

                        for idx in range(len(items) + 1):
                            if idx < len(items):
                                emit_scores(items[idx], idx % 4, idx % 2)
                            if idx >= 1:
                                emit_pv(items[idx - 1], (idx - 1) % 4)
                        for m in range(8):
                            pk = 'PS%d' % (pq % 2)
                            pst = PS[pq % 2]
                            pq += 1
                            for j in range(8):
                                P.op('pe', ['WO%d' % j, 'OT%d' % (j // 4)], [pk],
                                     mm(pst[:], WO[:, j, m * 128:(m + 1) * 128], OT[:, j // 4, j % 4, :], j == 0, j == 7))
                            P.op('dve', [pk, 'ADA', 'XT'], ['XT'],
                                 lambda e, pst=pst, m=m, cs=cs: e.scalar_tensor_tensor(
                                     out=XT[:, m, cs], in0=pst[:], scalar=ada_vec(l, 2, s, m), in1=XT[:, m, cs],
                                     op0=ALU.mult, op1=ALU.add))
                    P.barrier()

            def emit_ssm(i, l, s):
                with ExitStack() as es3:
                    UT = es3.enter_context(sb("UT", [128, 8, T], BF16))
                    WB = [es3.enter_context(sb("WB%d" % b, [128, 8, 256], BF16)) for b in range(2)]
                    SWB = [es3.enter_context(sb("SWB%d" % b, [128, 4096], BF16)) for b in range(2)]
                    SWK = [es3.enter_context(sb("SWK%d" % b, [128, 4608], BF16)) for b in range(2)]
                    SR = [[es3.enter_context(sb("SR%d%d" % (u, b), [128, 256], F32)) for b in range(2)] for u in range(2)]
                    SI = [[es3.enter_context(sb("SI%d%d" % (u, b), [128, 256], F32)) for b in range(2)] for u in range(2)]
                    XE = [es3.enter_context(sb("XE%d" % b, [128, 4, 2, 256], BF16)) for b in range(2)]
                    YA = es3.enter_context(sb("YA", [128, T], BF16))
                    G2 = es3.enter_context(sb("G2", [128, 256], F32))
                    G3 = es3.enter_context(sb("G3", [128, 256], F32))
                    G4 = es3.enter_context(sb("G4", [128, 256], F32))
                    SG = es3.enter_context(sb("SG", [128, 512], F32))
                    YF = es3.enter_context(sb("YF", [128, 512], F32))
                    for b in range(2):
                        P.op('pool', [], ['XE%d_%d%s' % (b, q_, ri_) for q_ in range(4) for ri_ in 'ri'], lambda e, b=b: e.memset(XE[b][:], 0.0))
                    wcnt = 0
                    pq = 0
                    for m in range(8):
                        b = wcnt % 2
                        wcnt += 1
                        P.dma('pool', WB[b][:, :, 0:128], w_in[i, :, :, m * 128:(m + 1) * 128], [], ['WBa%d' % b])
                        for blk in range(4):
                            cs = slice(blk * 512, (blk + 1) * 512)
                            pi = pq % 2
                            pq += 1
                            for kc in range(8):
                                P.op('pe', ['WBa%d' % b, 'HT%d_%d' % (kc, blk)], ['PS%d' % pi],
                                     mm(PS[pi][:], WB[b][:, kc, 0:128], HT[:, kc, cs], kc == 0, kc == 7))
                            P.op('act', ['PS%d' % pi], ['UT%d' % m],
                                 lambda e, m=m, cs=cs, pi=pi: e.copy(out=UT[:, m, cs], in_=PS[pi][:]))
                    def load_swb(cc):
                        P.dma('sp', SWB[cc % 2][:], ssm_scr[i, cc, :, 0:4096], ['SCR%d_%d' % (i, cc)], ['SWB%d' % (cc % 2)])

                    def load_swk(cc):
                        P.dma('sp', SWK[cc % 2][:], ssm_scr[i, cc, :, 4096:8704], ['SCR%d_%d' % (i, cc)], ['SWK%d' % (cc % 2)])

                    def emit_sn(cc):
                        swk = 'SWB%d' % (cc % 2)
                        BT8 = SWB[cc % 2][:, 0:4096].rearrange("p (t c) -> p t c", t=8)
                        for q in range(4):
                            h2, w = q // 2, q % 2
                            hs = slice(64 * h2, 64 * h2 + 64)
                            u = q % 2
                            pi = q % 2
                            for part in range(2):
                                for jj in range(8):
                                    P.op('pe', [swk, 'UT%d' % cc], ['PS%d' % pi],
                                         mm(PS[pi][:, part * 256:part * 256 + 256],
                                            BT8[hs, 7 - jj, w * 256 + part * 128:w * 256 + part * 128 + 128],
                                            UT[hs, cc, jj:T:8], jj == 0, jj == 7))
                            P.op('act', ['PS%d' % pi], ['SR%d0t' % u, 'SR%d0h' % u],
                                 lambda e, u=u, pi=pi: e.copy(out=SR[u][0][:], in_=PS[pi][:, 0:256]))
                            P.op('act', ['PS%d' % pi], ['SI%d0t' % u, 'SI%d0h' % u],
                                 lambda e, u=u, pi=pi: e.copy(out=SI[u][0][:], in_=PS[pi][:, 256:512]))
                            if q % 2 == 1:
                                gens = [emit_scan(cc, q - 1), emit_scan(cc, q)]
                                alive = True
                                while alive:
                                    alive = False
                                    for g_ in gens:
                                        try:
                                            next(g_)
                                            alive = True
                                        except StopIteration:
                                            pass

                    def emit_scan(cc, q):
                        j = cc * 4 + q
                        u = q % 2
                        NCH = 256
                        a = 0
                        for k in range(8):
                            d = 1 << k
                            b2 = 1 - a
                            ra, ia, rb, ib = 'SR%d%d' % (u, a), 'SI%d%d' % (u, a), 'SR%d%d' % (u, b2), 'SI%d%d' % (u, b2)
                            akr = AKS[:, i, 0, k, j:j + 1]
                            aki = AKS[:, i, 1, k, j:j + 1]
                            akn = AKS[:, i, 2, k, j:j + 1]
                            Ra, Ia, Rb, Ib = SR[u][a], SI[u][a], SR[u][b2], SI[u][b2]

                            def stt(out, in0, sc, in1, rk, wk):
                                P.op('dve', rk, wk, lambda e: e.scalar_tensor_tensor(
                                    out=out, in0=in0, scalar=sc, in1=in1, op0=ALU.mult, op1=ALU.add))
                            stt(Rb[:, d:NCH], Ra[:, 0:NCH - d], akr, Ra[:, d:NCH], [ra + 't', ra + 'h', 'AKS'], [rb + 't'])
                            stt(Rb[:, d:NCH], Ia[:, 0:NCH - d], akn, Rb[:, d:NCH], [ia + 't', ia + 'h', rb + 't', 'AKS'], [rb + 't'])
                            stt(Ib[:, d:NCH], Ia[:, 0:NCH - d], akr, Ia[:, d:NCH], [ia + 't', ia + 'h', 'AKS'], [ib + 't'])
                            stt(Ib[:, d:NCH], Ra[:, 0:NCH - d], aki, Ib[:, d:NCH], [ra + 't', ra + 'h', ib + 't', 'AKS'], [ib + 't'])
                            P.op('pool', [ra + 't', ra + 'h'], [rb + 'h'], lambda e, Ra=Ra, Rb=Rb, d=d: e.tensor_copy(out=Rb[:, 0:d], in_=Ra[:, 0:d]))
                            P.op('act', [ia + 't', ia + 'h'], [ib + 'h'], lambda e, Ia=Ia, Ib=Ib, d=d: e.copy(out=Ib[:, 0:d], in_=Ia[:, 0:d]))
                            a = b2
                            yield
                        xe = XE[cc % 2]
                        xk = 'XE%d_%d' % (cc % 2, q)
                        P.op('act', ['SR%d%dt' % (u, a), 'SR%d%dh' % (u, a)], [xk + 'r'],
                             lambda e, xe=xe, q=q, u=u, a=a: e.copy(out=xe[:, q, 0, 1:NCH], in_=SR[u][a][:, 0:NCH - 1]))
                        P.op('pool', ['SI%d%dt' % (u, a), 'SI%d%dh' % (u, a)], [xk + 'i'],
                             lambda e, xe=xe, q=q, u=u, a=a: e.tensor_copy(out=xe[:, q, 1, 1:NCH], in_=SI[u][a][:, 0:NCH - 1]))

                    def emit_y(cc):
                        swk = 'SWK%d' % (cc % 2)
                        KT = SWK[cc % 2][:, 0:512].rearrange("p (t c) -> p t c", t=8)
                        CA = SWK[cc % 2][:, 512:4608].rearrange("p (r t q c) -> p r t q c", r=2, t=8, q=4)
                        xe = XE[cc % 2]
                        for t in range(8):
                            pi = 4 + (t % 4)
                            psy = PS[pi]
                            for h2 in range(2):
                                hs = slice(64 * h2, 64 * h2 + 64)
                                pk = 'PS%d_%d' % (pi, h2)
                                for jj in range(t + 1):
                                    P.op('pe', [swk, 'UT%d' % cc], [pk],
                                         mm(psy[hs, 0:256], KT[hs, t - jj, :], UT[hs, cc, jj:T:8], jj == 0, False))
                                for w in range(2):
                                    q = 2 * h2 + w
                                    xk = 'XE%d_%d' % (cc % 2, q)
                                    P.op('pe', [swk, xk + 'r'], [pk],
                                         mm(psy[hs, 0:256], CA[:, 0, t, q, :], xe[:, q, 0, :], False, False))
                                    P.op('pe', [swk, xk + 'i'], [pk],
                                         mm(psy[hs, 0:256], CA[:, 1, t, q, :], xe[:, q, 1, :], False, w == 1))
                            pks = ['PS%d_0' % pi, 'PS%d_1' % pi]
                            P.op('act', pks, ['G2'], lambda e, psy=psy: e.activation(out=G2[:], in_=psy[:, 0:256], func=AF.Square))
                            P.op('dve', ['G2'], ['G3'], lambda e: e.tensor_scalar(
                                out=G3[:], in0=G2[:], scalar1=0.044715, scalar2=1.0, op0=ALU.mult, op1=ALU.add))
                            P.op('dve', ['G3'] + pks, ['G4'], lambda e, psy=psy: e.tensor_tensor(
                                out=G4[:], in0=G3[:], in1=psy[:, 0:256], op=ALU.mult))
                            P.op('act', ['G4'], ['G2'], lambda e: e.activation(out=G2[:], in_=G4[:], func=AF.Sigmoid, scale=1.5957691216057308))
                            P.op('dve', ['G2'] + pks, ['YA'], lambda e, psy=psy, t=t: e.tensor_tensor(
                                out=YA[:, t:T:8], in0=G2[:], in1=psy[:, 0:256], op=ALU.mult))
                        P.op('act', ['YA'], ['UT%d' % cc], lambda e, cc=cc: e.copy(out=UT[:, cc, :], in_=YA[:]))

                    load_swb(0)
                    for cc in range(8):
                        if cc + 1 < 8:
                            load_swb(cc + 1)
                        load_swk(cc)
                        emit_sn(cc)
                        if cc >= 1:
                            emit_y(cc - 1)
                    emit_y(7)
                    for m in range(8):
                        b = wcnt % 2
                        wcnt += 1
                        P.dma('pool', WB[b][:, :, 0:128], w_glu[i, :, :, m * 128:(m + 1) * 128], [], ['WBa%d' % b])
                        P.dma('pool', WB[b][:, :, 128:256], w_glu[i, :, :, D + m * 128:D + (m + 1) * 128], [], ['WBb%d' % b])
                        for blk in range(4):
                            cs = slice(blk * 512, (blk + 1) * 512)
                            uk = ['UT%d' % kc for kc in range(8)]
                            pa, pb_ = (0, 1) if (blk % 2 == 0) else (2, 3)
                            PA, PB_ = PS[pa], PS[pb_]
                            for kc in range(8):
                                P.op('pe', ['WBa%d' % b] + uk, ['PS%d' % pa], mm(PA[:], WB[b][:, kc, 0:128], UT[:, kc, cs], kc == 0, kc == 7))
                            for kc in range(8):
                                P.op('pe', ['WBb%d' % b] + uk, ['PS%d' % pb_], mm(PB_[:], WB[b][:, kc, 128:256], UT[:, kc, cs], kc == 0, kc == 7))
                            P.op('act', ['PS%d' % pb_], ['SG'], lambda e, PB_=PB_: e.activation(out=SG[:], in_=PB_[:], func=AF.Sigmoid))
                            P.op('dve', ['PS%d' % pa, 'SG'], ['YF'], lambda e, PA=PA: e.tensor_tensor(out=YF[:], in0=PA[:], in1=SG[:], op=ALU.mult))
                            P.op('dve', ['YF', 'ADA', 'XT%d_%d' % (m, blk)], ['XT%d_%d' % (m, blk)],
                                 lambda e, m=m, cs=cs: e.scalar_tensor_tensor(
                                     out=XT[:, m, cs], in0=YF[:], scalar=ada_vec(l, 2, s, m), in1=XT[:, m, cs],
                                     op0=ALU.mult, op1=ALU.add))
                    P.barrier()

            def emit_moe(l, s):
                with ExitStack() as es:
                    WGU = [es.enter_context(sb("WGU%d" % b, [128, 8, 512], BF16)) for b in range(2)]
                    WDN = [es.enter_context(sb("WDN%d" % b, [128, 2, D], BF16)) for b in range(2)]
                    SEL = es.enter_context(sb("SEL", [32, 32, 128], F32))
                    GBS = [es.enter_context(sb("GBS%d" % b, [128, 512], BF16)) for b in range(2)]
                    TS = [es.enter_context(sb("TS%d" % b, [128, 2, 512], BF16)) for b in range(2)]
                    T2 = [es.enter_context(sb("T2%d" % b, [128, 2, 512], BF16)) for b in range(2)]
                    HID = [es.enter_context(sb("HID%d" % b, [128, 2, 512], BF16)) for b in range(2)]
                    P.dma('sp', SEL[:], sel[:, :, :], [], ['SEL'])

                    def load_w(e):
                        b = e % 2
                        for kc in range(8):
                            P.dma('pool', WGU[b][:, kc, :], wgu[l, e, :, kc, :], [], ['WGU%d' % b])
                        for f in range(2):
                            P.dma('pool', WDN[b][:, f, :], wdn[l, e, :, f, :], [], ['WDN%d' % b])
                    load_w(0)
                    items = [(e, blk) for e in range(NE) for blk in range(4)]
                    ycnt = [0]

                    def emit_gu(it, k):
                        e, blk = it
                        b = e % 2
                        r = k % 2
                        cs = slice(blk * 512, (blk + 1) * 512)
                        hkeys = ['HT%d_%d' % (kc, blk) for kc in range(8)]
                        P.op('pe', ['SEL', 'GT%d' % blk], ['PS4'], mm(PS4[:], SEL[:, e, :], GT[:, cs], True, True))
                        P.op('act', ['PS4'], ['GBS%d' % r], lambda en: en.copy(out=GBS[r][:], in_=PS4[:]))
                        for f in range(2):
                            for kc in range(8):
                                P.op('pe', ['WGU%d' % b] + hkeys, ['PS%d' % f],
                                     mm(PS[f][:], WGU[b][:, kc, f * 128:(f + 1) * 128], HT[:, kc, cs], kc == 0, kc == 7))
                            for kc in range(8):
                                P.op('pe', ['WGU%d' % b] + hkeys, ['PS%d' % (2 + f)],
                                     mm(PS[2 + f][:], WGU[b][:, kc, 256 + f * 128:256 + (f + 1) * 128], HT[:, kc, cs],
                                        kc == 0, kc == 7))
                            P.op('act', ['PS%d' % f], ['TS%d_%d' % (r, f)],
                                 lambda en, f=f: en.activation(out=TS[r][:, f, :], in_=PS[f][:], func=AF.Silu))
                            P.op('pool', ['TS%d_%d' % (r, f), 'GBS%d' % r], ['T2%d_%d' % (r, f)],
                                 lambda en, f=f: en.tensor_tensor(out=T2[r][:, f, :], in0=TS[r][:, f, :],
                                                                  in1=GBS[r][:], op=ALU.mult))
                            P.op('dve', ['PS%d' % (2 + f), 'T2%d_%d' % (r, f)], ['HID%d_%d' % (r, f)],
                                 lambda en, f=f: en.tensor_tensor(out=HID[r][:, f, :], in0=PS[2 + f][:],
                                                                  in1=T2[r][:, f, :], op=ALU.mult))

                    def emit_y(it, k):
                        e, blk = it
                        b = e % 2
                        r = k % 2
                        cs = slice(blk * 512, (blk + 1) * 512)
                        for m in range(8):
                            pi = 5 + (ycnt[0] % 3)
                            ycnt[0] += 1
                            for f in range(2):
                                P.op('pe', ['WDN%d' % b, 'HID%d_%d' % (r, f)], ['PS%d' % pi],
                                     mm(PS[pi][:], WDN[b][:, f, m * 128:(m + 1) * 128], HID[r][:, f, :], f == 0, f == 1))
                            P.op('dve', ['PS%d' % pi, 'ADA', 'XT%d_%d' % (m, blk)], ['XT%d_%d' % (m, blk)],
                                 lambda en, m=m, pi=pi: en.scalar_tensor_tensor(
                                     out=XT[:, m, cs], in0=PS[pi][:], scalar=ada_vec(l, 5, s, m), in1=XT[:, m, cs],
                                     op0=ALU.mult, op1=ALU.add))

                    for k in range(len(items) + 1):
                        if k < len(items):
                            emit_gu(items[k], k)
                        if k >= 1:
                            emit_y(items[k - 1], k - 1)
                        if k < len(items) and items[k][1] == 0 and items[k][0] + 1 < NE:
                            load_w(items[k][0] + 1)
                    P.barrier()

            def emit_moe_sparse(l, s):
                NB = 64
                with ExitStack() as es:
                    WGU = [es.enter_context(sb("WGU%d" % b, [128, 8, 512], BF16)) for b in range(3)]
                    WDN = [es.enter_context(sb("WDN%d" % b, [128, 2, D], BF16)) for b in range(3)]
                    XB = [es.enter_context(sb("XB%d" % b, [128, 1024], BF16)) for b in range(2)]
                    XBT = [es.enter_context(sb("XBT%d" % b, [128, 8, 128], BF16)) for b in range(2)]
                    TS = [es.enter_context(sb("TS%d" % b, [128, 256], BF16)) for b in range(2)]
                    HID = [es.enter_context(sb("HID%d" % b, [128, 256], BF16)) for b in range(2)]
                    YR = [es.enter_context(sb("YR%d" % b, [128, 1024], F32)) for b in range(2)]
                    YTOK = [es.enter_context(sb("YTOK%d" % b, [128, 1024], F32)) for b in range(4)]
                    YTK2 = [es.enter_context(sb("YTK2%d" % b, [128, 1024], F32)) for b in range(4)]
                    PSB = PS7[:].bitcast(BF16)
                    IDXGF = IDXG[:].bitcast(F32)
                    if 'yd' not in _regs:
                        _regs['yd'] = nc.gpsimd.alloc_register(name="ydmax_reg")
                        nc.gpsimd.reg_mov(_regs['yd'], 8191)
                    if 'hd' not in _regs:
                        _regs['hd'] = nc.gpsimd.alloc_register(name="hdmax_reg")
                        nc.gpsimd.reg_mov(_regs['hd'], 2175)
                    if 'wmax' not in _regs:
                        _regs['wmax'] = nc.gpsimd.alloc_register(name="wmax_reg")
                        nc.gpsimd.reg_mov(_regs['wmax'], L * NE * 128 - 1)
                    WMAX = _regs['wmax']

                    def fetch_x(i, b):
                        r = i % 2
                        P.idma('pool', lambda g: g.indirect_dma_start(
                            out=XB[r][:], out_offset=None, in_=HD[:, :],
                            in_offset=bass.IndirectOffsetOnAxis(ap=IDXG[:, b, 0:1], axis=0),
                            bounds_check=_regs['hd'], oob_is_err=False), ['IDXG', 'HD'], ['XB%d' % r])

                    def fetch(i, b):
                        w3 = i % 3
                        for (dst, srcw, hk) in ((WGU[w3][:, 0:4, :].rearrange("p a c -> p (a c)"), wguA, 'a'),
                                                (WGU[w3][:, 4:8, :].rearrange("p a c -> p (a c)"), wguB, 'b')):
                            P.idma('pool', lambda g, dst=dst, srcw=srcw: g.indirect_dma_start(
                                out=dst, out_offset=None, in_=srcw[:, :],
                                in_offset=bass.IndirectOffsetOnAxis(ap=IDXW[:, b:b + 1], axis=0),
                                bounds_check=WMAX, oob_is_err=False), ['IDXW'], ['WGU%s%d' % (hk, w3)])
                        P.idma('pool', lambda g: g.indirect_dma_start(
                            out=WDN[w3][:].rearrange("p a c -> p (a c)"), out_offset=None, in_=wdn2[:, :],
                            in_offset=bass.IndirectOffsetOnAxis(ap=IDXW[:, b:b + 1], axis=0),
                            bounds_check=WMAX, oob_is_err=False), ['IDXW'], ['WDN%d' % w3])

                    def emit_gu(i, b):
                        r = i % 2
                        w3 = i % 3
                        for kc in range(8):
                            P.op('pe', ['XB%d' % r, 'IDB'], ['PS7'],
                                 lambda pe, kc=kc: pe.transpose(PSB[:, kc * 128:(kc + 1) * 128], XB[r][:, kc * 128:(kc + 1) * 128], IDB[:]))
                        P.op('act', ['PS7'], ['XBT%d' % r], lambda e: e.copy(out=XBT[r][:].rearrange("p a c -> p (a c)"), in_=PSB))
                        pk = 'PS%d' % r
                        for grp in range(4):
                            for kc in range(8):
                                P.op('pe', ['WGUa%d' % w3, 'WGUb%d' % w3, 'XBT%d' % r], [pk],
                                     mm(PS[r][:, grp * 128:(grp + 1) * 128], WGU[w3][:, kc, grp * 128:(grp + 1) * 128],
                                        XBT[r][:, kc, :], kc == 0, kc == 7))
                        P.op('act', [pk], ['TS%d' % r], lambda e: e.activation(out=TS[r][:], in_=PS[r][:, 0:256], func=AF.Silu))
                        P.op('dve', [pk, 'TS%d' % r], ['HID%d' % r], lambda e: e.tensor_tensor(
                            out=HID[r][:], in0=PS[r][:, 256:512], in1=TS[r][:], op=ALU.mult))

                    def emit_y(i, b):
                        r = i % 2
                        w3 = i % 3
                        for hf in range(2):
                            pi = 2 + 2 * r + hf
                            for f in range(2):
                                P.op('pe', ['WDN%d' % w3, 'HID%d' % r], ['PS%d' % pi],
                                     mm(PS[pi][:], HID[r][:, f * 128:(f + 1) * 128], WDN[w3][:, f, hf * 512:(hf + 1) * 512], f == 0, f == 1))
                            if hf == 0:
                                P.op('act', ['PS%d' % pi, 'IDXG'], ['YR%d_0' % r], lambda e, pi=pi: e.activation(
                                    out=YR[r][:, 0:512], in_=PS[pi][:], func=AF.Identity, scale=IDXGF[:, b, 1:2]))
                            else:
                                P.op('dve', ['PS%d' % pi, 'IDXG'], ['YR%d_1' % r], lambda e, pi=pi: e.tensor_scalar(
                                    out=YR[r][:, 512:1024], in0=PS[pi][:], scalar1=IDXGF[:, b, 1:2], scalar2=None, op0=ALU.mult))
                        P.dma('sp', YD[b * 128:(b + 1) * 128, :], YR[r][:], ['YR%d_0' % r, 'YR%d_1' % r], ['YD%d' % b])

                    seq = []
                    for k in range(MOE_R1):
                        seq += [k, MOE_R0 + k, MOE_R0 + MOE_R1 + k]
                    seq += list(range(MOE_R1, MOE_R0))
                    assert sorted(seq) == list(range(NB)) and MOE_R0 - MOE_R1 <= 1 and NB - MOE_R0 - MOE_R1 == MOE_R1
                    pos = list(enumerate(seq))
                    fetch_x(*pos[0])
                    fetch_x(*pos[1])
                    fetch(*pos[0])
                    fetch(*pos[1])
                    fetch(*pos[2])
                    for j in range(NB + 1):
                        if j < NB:
                            emit_gu(*pos[j])
                            if j + 2 < NB:
                                fetch_x(*pos[j + 2])
                        if j >= 1:
                            emit_y(*pos[j - 1])
                            if j + 2 < NB:
                                fetch(*pos[j + 2])
                    cnt = 0
                    for n in range(16):
                        yb = n % 4
                        ydk = ['YD%d' % b_ for b_ in range(NB)]
                        for (dstt, kk_, nm) in ((YTOK[yb], 0, ['YTOKa%d' % yb, 'YTOK%d' % yb]), (YTK2[yb], 1, ['YTOKb%d' % yb])):
                            P.idma('pool', lambda g, dstt=dstt, kk_=kk_: g.indirect_dma_start(
                                out=dstt[:], out_offset=None, in_=YD[:, :],
                                in_offset=bass.IndirectOffsetOnAxis(ap=DSTI[:, 2 * n + kk_:2 * n + kk_ + 1], axis=0),
                                bounds_check=_regs['yd'], oob_is_err=False), ydk + ['DSTI'], nm)
                        P.op('dve', ['YTOKa%d' % yb, 'YTOKb%d' % yb, 'YTOK%d' % yb], ['YTOK%d' % yb], lambda e, yb=yb: e.tensor_tensor(
                            out=YTOK[yb][:], in0=YTOK[yb][:], in1=YTK2[yb][:], op=ALU.add))
                        for m in range(8):
                            pi = cnt % 4
                            cnt += 1
                            P.op('pe', ['YTOK%d' % yb, 'IDF'], ['PS%d' % pi],
                                 lambda pe, m=m, pi=pi, yb=yb: pe.transpose(PS[pi][:, 0:128], YTOK[yb][:, m * 128:(m + 1) * 128], IDF[:]))
                            P.op('dve', ['PS%d' % pi, 'ADA', 'XT%d' % m], ['XT%d' % m],
                                 lambda e, m=m, pi=pi, n=n: e.scalar_tensor_tensor(
                                     out=XT[:, m, n * 128:(n + 1) * 128], in0=PS[pi][:, 0:128], scalar=ada_vec(l, 5, s, m),
                                     in1=XT[:, m, n * 128:(n + 1) * 128], op0=ALU.mult, op1=ALU.add))
                    P.barrier()

            if SPARSE:
                with ExitStack() as esz:
                    ZB = esz.enter_context(sb("ZB", [128, 1024], BF16))
                    P.op('dve', [], ['ZB'], lambda e: e.memset(ZB[:], 0.0))
                    P.dma('sp', HD[2048:2176, :], ZB[:], ['ZB'], ['HDz'])
                    P.barrier()
            for s in range(n_seq):
                for kc in range(8):
                    P.dma('sp', XT[:, kc, :], xT[s, kc * 128:(kc + 1) * 128, :], [], ['XTL%d' % kc])
                for l in layers:
                    if l % 2 == 0:
                        with ExitStack() as esa:
                            WQ = esa.enter_context(sb("WQ", [128, 8, 1536], BF16))
                            WO = esa.enter_context(sb("WO", [128, 8, D], BF16))
                            for kc in range(8):
                                P.dma('pool', WQ[:, kc, :], wqkv[l // 2, :, kc, :], [], ['WQ%d' % kc])
                            for kc in range(8):
                                P.dma('pool', WO[:, kc, :], wo[l // 2, :, kc, :], [], ['WO%d' % kc])
                            emit_norm(l, s, 0)
                            emit_attn(l // 2, l, s)
                    else:
                        emit_norm(l, s, 0)
                        emit_ssm(l // 2, l, s)
                    if dbg != 'mix':
                        with ExitStack() as esg:
                            if SPARSE:
                                IDXG = esg.enter_context(sb("IDXG", [128, 64, 8], I32))
                                IDXW = esg.enter_context(sb("IDXW", [128, 64], I32))
                                DSTI = esg.enter_context(sb("DSTI", [128, 32], I32))
                                emit_norm(l, s, 1)
                                emit_moe_sparse(l, s)
                            else:
                                GT = esg.enter_context(sb("GT", [32, T], F32))
                                emit_norm(l, s, 1)
                                emit_moe(l, s)
                    P.barrier()
                for kc in range(8):
                    P.dma('sp', yT[s, kc * 128:(kc + 1) * 128, :], XT[:, kc, :], ['XTL%d' % kc], ['yT%d' % kc])
            P.barrier()
    return nc


def _pmajor(w, kc):
    sh = w.shape
    w = w.reshape(sh[:-2] + (kc, 128, sh[-1]))
    nd = w.ndim
    perm = list(range(nd - 3)) + [nd - 2, nd - 3, nd - 1]
    return np.ascontiguousarray(w.transpose(perm))


def prep_shared(inp):
    f = np.float32
    sh = {}
    sh['gmix'] = np.ascontiguousarray(inp['norm_mix'].reshape(L, 8, 128).transpose(2, 0, 1)).astype(f)
    sh['gffn'] = np.ascontiguousarray(inp['norm_ffn'].reshape(L, 8, 128).transpose(2, 0, 1)).astype(f)
    sh['w_ada'] = _pmajor(inp['w_ada'], 8)
    sh['b_ada'] = np.ascontiguousarray(inp['b_ada'].reshape(L, 48, 128).transpose(2, 0, 1))
    cols = []
    for c in range(8):
        for s2 in range(2):
            hd = 4 * (2 * (c // 4) + s2) + (c % 4)
            cols += list(range(hd * 64, hd * 64 + 64))
    cols += list(range(1024, 1536))
    sh['wqkv'] = _pmajor(inp['attn_w_qkv'][:, :, cols], 8)
    sh['qgain'] = np.ascontiguousarray(np.tile(inp['attn_q_gain'], (1, 2)).T)
    sh['kgain'] = np.ascontiguousarray(np.tile(inp['attn_k_gain'], (1, 2)).T)
    sk = np.zeros((2, 128, 2, 512), f)
    for i2 in range(2):
        for hh in range(2):
            h = 2 * i2 + hh
            for g in range(4):
                sk[:, 64 * hh:64 * hh + 64, i2, g * 128:(g + 1) * 128] = inp['attn_sinks'][:, 4 * h + g][:, None, None]
    sh['sinks'] = sk
    rows = []
    for i2 in range(2):
        for g in range(4):
            for hh in range(2):
                hd = 4 * (2 * i2 + hh) + g
                rows += list(range(hd * 64, hd * 64 + 64))
    sh['wo'] = _pmajor(inp['attn_w_o'][:, rows, :], 8)
    mb = np.zeros((128, 2, 4, 128), f)
    sidx = np.arange(128)[:, None]
    qidx = np.arange(128)[None, :]
    mb[:, 0] = np.where(sidx > qidx, 0.0, NEG)[:, None, :]
    mb[:, 1] = np.where(sidx <= qidx, 0.0, NEG)[:, None, :]
    sh['maskb'] = mb.reshape(128, 2, 512).astype(ml_dtypes.bfloat16)
    sh['identb'] = np.eye(128, dtype=f).astype(ml_dtypes.bfloat16)
    ob = np.zeros((128, 128), f)
    ob[:64, :64] = 1
    ob[64:, 64:] = 1
    sh['onesblk'] = ob.astype(ml_dtypes.bfloat16)
    sh['identf'] = np.eye(128, dtype=f)
    sh['ident2'] = np.concatenate([np.eye(64, dtype=f), np.eye(64, dtype=f)], axis=0)
    sh['wr'] = _pmajor(np.concatenate([inp['moe_w_group'], inp['moe_w_expert']], axis=-1), 8)
    sh['br'] = np.ascontiguousarray(np.concatenate([inp['moe_b_group'], inp['moe_b_expert']], axis=-1)[None])
    wgu_p = _pmajor(np.concatenate([inp['moe_w_gate'], inp['moe_w_up']], axis=-1), 8).reshape(L * NE * 128, 4096)
    sh['wguA'] = np.ascontiguousarray(wgu_p[:, 0:2048])
    sh['wguB'] = np.ascontiguousarray(wgu_p[:, 2048:4096])
    del wgu_p
    sh['wdn2'] = _pmajor(inp['moe_w_down'], 2).reshape(L * NE * 128, 2048)
    kk = np.arange(128)
    sh['trib'] = (kk[:, None] < kk[None, :]).astype(f).astype(ml_dtypes.bfloat16)
    sh['toki'] = (np.arange(16)[None, :] * 128 + kk[:, None]).astype(np.int32)
    ri = np.zeros((128, 64, 8), np.int32)
    ri[:, :, 0] = 2048 + kk[:, None]
    sh['rinit'] = ri.reshape(128, 512)
    sh['b128'] = np.broadcast_to((np.arange(64) * 128).astype(f)[None, :], (128, 64)).copy()
    sh['pidx'] = kk.astype(f)[:, None].copy()
    sh['w_in'] = _pmajor(inp['ssm_w_in'], 8)
    sh['w_glu'] = _pmajor(inp['ssm_w_glu'], 8)
    sh['ssm_d'] = np.ascontiguousarray(inp['ssm_d'].reshape(2, 8, 128).transpose(2, 0, 1))

    def gp(a):
        return np.ascontiguousarray(a.reshape(2, 32, 2, 64).transpose(2, 3, 0, 1).reshape(128, 2, 32))
    sh['lam_re'] = gp(inp['ssm_lam_re'])
    sh['lam_im'] = gp(inp['ssm_lam_im'])
    sh['log_dt'] = gp(np.broadcast_to(inp['ssm_log_dt'][:, :, None], (2, 64, 64)))

    def blk(a):
        o = np.zeros((128, 2, 32, 64), f)
        a5 = a.reshape(2, 32, 2, 64, 16)
        for g2 in range(2):
            for w in range(2):
                o[64 * g2:64 * g2 + 64, :, w::2, 32 * w + 16 * g2:32 * w + 16 * g2 + 16] = \
                    a5[:, w::2, g2].transpose(2, 0, 1, 3)
        return o
    sh['b_re'] = blk(inp['ssm_b_re'])
    sh['b_im'] = blk(inp['ssm_b_im'])
    sh['c_re'] = blk(inp['ssm_c_re'].transpose(0, 1, 3, 2))
    sh['c_im'] = blk(inp['ssm_c_im'].transpose(0, 1, 3, 2))
    return {k: np.ascontiguousarray(v) for k, v in sh.items()}


def prep_core(inp, core):
    b0 = core * SEQ_PER_CORE
    x = inp['x'][b0:b0 + SEQ_PER_CORE]
    xTn = np.ascontiguousarray(x.transpose(0, 2, 1))
    c = inp['c'][b0:b0 + SEQ_PER_CORE]
    cTn = np.ascontiguousarray(c.reshape(SEQ_PER_CORE, 8, 128).transpose(2, 1, 0))
    return {'xT': xTn, 'cT': cTn}


_CACHE = {}


def kernel(**inputs):
    inp = {k: np.asarray(v) for k, v in inputs.items()}
    if 'nc' not in _CACHE:
        _CACHE['nc'] = build_program()
    nc = _CACHE['nc']
    shared = prep_shared(inp)
    in_maps = []
    for core in range(NCORES):
        m = dict(shared)
        m.update(prep_core(inp, core))
        in_maps.append(m)
    res = run_bass_kernel_spmd(nc, in_maps, core_ids=list(range(NCORES)))
    out = np.empty((NCORES * SEQ_PER_CORE, T, D), np.float32)
    for core in range(NCORES):
        yT = res.results[core]['yT']
        for s in range(SEQ_PER_CORE):
            out[core * SEQ_PER_CORE + s] = yT[s].T
    return out
```
